# Optimizing a Trainium2 kernel written in Bass

```python
import math
import jax, jax.numpy as jnp
from jax import lax
import numpy as np


D_MODEL = 4096
BATCH = 1
SEQ = 16384
DEPTH = 4

DIFF_HEADS = 8
DIFF_HEAD_DIM = 128
DIFF_WIDTH = DIFF_HEADS * 2 * DIFF_HEAD_DIM
Q_BLOCK = 128
REL_BUCKETS = 32
REL_MAX_DIST = 128
RET_HEADS = 8
RET_HEAD_DIM = 128
RET_WIDTH = RET_HEADS * RET_HEAD_DIM
RET_CHUNK = 128
S5_WIDTH = 1024
S5_GROUP = 16
S5_GROUPS = S5_WIDTH // S5_GROUP
S5_STATE = 64
N_BRANCHES = 3
GATE_RANK = 256
FFN_HIDDEN = ((8 * D_MODEL + 767) // 768) * 256
PLE_DIM = 256
EPS = 1e-6

OFF_DQ = 0
OFF_DK = OFF_DQ + DIFF_WIDTH
OFF_DV = OFF_DK + DIFF_WIDTH
OFF_RQ = OFF_DV + DIFF_WIDTH
OFF_RK = OFF_RQ + RET_WIDTH
OFF_RV = OFF_RK + RET_WIDTH
OFF_RG = OFF_RV + RET_WIDTH
OFF_SU = OFF_RG + RET_WIDTH
OFF_GATE = OFF_SU + S5_WIDTH
IN_WIDTH = OFF_GATE + GATE_RANK

kernel_name = "hybrid_diffattn_retention_s5_gated_trunk"


def rms_norm(x, gain):
    xf = x.astype(jnp.float32)
    y = xf * lax.rsqrt(jnp.mean(xf * xf, axis=-1, keepdims=True) + EPS)
    return (y * gain.astype(jnp.float32)).astype(x.dtype)


def t5_bucket(rel):
    n = jnp.maximum(rel, 0)
    max_exact = REL_BUCKETS // 2
    nf = jnp.maximum(n, 1).astype(jnp.float32)
    large = max_exact + (jnp.log(nf / max_exact) / math.log(REL_MAX_DIST / max_exact)
                         * (REL_BUCKETS - max_exact)).astype(jnp.int32)
    large = jnp.minimum(large, REL_BUCKETS - 1)
    return jnp.where(n < max_exact, n, large)


def diff_attention(q, k, v, rel_bias, lam, subln, lam_init):
    B, S = q.shape[0], q.shape[1]
    nb = S // Q_BLOCK
    scale = DIFF_HEAD_DIM ** -0.5
    k_pos = jnp.arange(S)
    q_blocks = q.reshape(B, nb, Q_BLOCK, DIFF_HEADS, 2, DIFF_HEAD_DIM).transpose(1, 0, 2, 3, 4, 5)

    def block(args):
        qb, bi = args
        q_pos = bi * Q_BLOCK + jnp.arange(Q_BLOCK)
        rel = q_pos[:, None] - k_pos[None, :]
        bias = jnp.transpose(rel_bias[t5_bucket(rel)], (2, 0, 1)).astype(jnp.float32)
        s = jnp.einsum('bqhcd,bkhcd->bchqk', qb, k).astype(jnp.float32) * scale + bias
        s = jnp.where(rel >= 0, s, -jnp.inf)
        pr = jax.nn.softmax(s, axis=-1)
        a = pr[:, 0] - lam * pr[:, 1]
        return jnp.einsum('bhqk,bkhe->bqhe', a.astype(v.dtype), v)

    out = lax.map(block, (q_blocks, jnp.arange(nb)))
    out = out.transpose(1, 0, 2, 3, 4).reshape(B, S, DIFF_HEADS, 2 * DIFF_HEAD_DIM)
    out = rms_norm(out, subln) * (1.0 - lam_init)
    return out.reshape(B, S, DIFF_WIDTH)


def retention(q, k, v, g):
    B, S = q.shape[0], q.shape[1]
    f32 = jnp.float32
    half = RET_HEAD_DIM // 2
    pos = jnp.arange(S, dtype=f32)
    theta = 1.0 / (10000.0 ** jnp.linspace(0.0, 1.0, half, dtype=f32))
    ang = pos[:, None] * theta[None, :]
    cos = jnp.cos(ang)[None, :, None, :]
    sin = jnp.sin(ang)[None, :, None, :]

    def rot(t):
        t1, t2 = t[..., :half], t[..., half:]
        return jnp.concatenate([t1 * cos - t2 * sin, t1 * sin + t2 * cos], axis=-1)

    q = rot(q.astype(f32))
    k = rot(k.astype(f32)) * (RET_HEAD_DIM ** -0.5)
    v = v.astype(f32)

    log_gamma = jnp.log1p(-jnp.exp2(-5.0 - jnp.arange(RET_HEADS, dtype=f32)))
    idx = jnp.arange(RET_CHUNK)
    diff = idx[:, None] - idx[None, :]
    dmat = jnp.where(diff >= 0,
                     jnp.exp(log_gamma[:, None, None] * jnp.maximum(diff, 0).astype(f32)), 0.0)
    xi = jnp.exp(log_gamma[None, :] * (idx[:, None] + 1).astype(f32))
    zeta = jnp.exp(log_gamma[None, :] * (RET_CHUNK - 1 - idx)[:, None].astype(f32))
    chunk_decay = jnp.exp(log_gamma * RET_CHUNK)

    nc = S // RET_CHUNK

    def chunks(t):
        return t.reshape(B, nc, RET_CHUNK, RET_HEADS, RET_HEAD_DIM).transpose(1, 0, 2, 3, 4)

    def step(R, xs):
        qc, kc, vc = xs
        inner = jnp.einsum('bihd,bjhd->bhij', qc, kc) * dmat[None]
        o = (jnp.einsum('bhij,bjhe->bihe', inner, vc)
             + jnp.einsum('bihd,bhde->bihe', qc, R) * xi[None, :, :, None])
        R = (R * chunk_decay[None, :, None, None]
             + jnp.einsum('bjhd,bjhe->bhde', kc * zeta[None, :, :, None], vc))
        return R, o

    R0 = jnp.zeros((B, RET_HEADS, RET_HEAD_DIM, RET_HEAD_DIM), f32)
    _, o = lax.scan(step, R0, (chunks(q), chunks(k), chunks(v)))
    o = o.transpose(1, 0, 2, 3, 4).reshape(B, S, RET_HEADS, RET_HEAD_DIM)
    o = o * lax.rsqrt(jnp.mean(o * o, axis=-1, keepdims=True) + EPS)
    return jax.nn.silu(g.astype(f32)) * o.reshape(B, S, RET_WIDTH)


def s5_scan_op(e1, e2):
    a1r, a1i, b1r, b1i = e1
    a2r, a2i, b2r, b2i = e2
    return (a1r * a2r - a1i * a2i,
            a1r * a2i + a1i * a2r,
            a2r * b1r - a2i * b1i + b2r,
            a2r * b1i + a2i * b1r + b2i)


def s5_layer(u, lam_re, lam_im, log_dt, b_re, b_im, c_re, c_im, d_skip, w_glu):
    B, S = u.shape[0], u.shape[1]
    f32 = jnp.float32
    uf = u.astype(f32).reshape(B, S, S5_GROUPS, S5_GROUP)
    lr, li = lam_re.astype(f32), lam_im.astype(f32)
    dt = jnp.exp(log_dt.astype(f32))[:, None]
    mag = jnp.exp(lr * dt)
    ar = mag * jnp.cos(li * dt)
    ai = mag * jnp.sin(li * dt)
    den = lr * lr + li * li
    fr = ((ar - 1.0) * lr + ai * li) / den
    fi = (ai * lr - (ar - 1.0) * li) / den
    br_, bi_ = b_re.astype(f32), b_im.astype(f32)
    bbr = fr[..., None] * br_ - fi[..., None] * bi_
    bbi = fr[..., None] * bi_ + fi[..., None] * br_
    xr = jnp.einsum('bsgj,gnj->bsgn', uf, bbr)
    xim = jnp.einsum('bsgj,gnj->bsgn', uf, bbi)
    ar_t = jnp.broadcast_to(ar, xr.shape)
    ai_t = jnp.broadcast_to(ai, xr.shape)
    _, _, hr, hi = lax.associative_scan(s5_scan_op, (ar_t, ai_t, xr, xim), axis=1)
    y = (jnp.einsum('bsgn,gjn->bsgj', hr, c_re.astype(f32))
         - jnp.einsum('bsgn,gjn->bsgj', hi, c_im.astype(f32))
         + d_skip.astype(f32).reshape(S5_GROUPS, S5_GROUP) * uf)
    z = jax.nn.gelu(y.reshape(B, S, S5_WIDTH))
    zg = z @ w_glu.astype(f32)
    return zg[..., :S5_WIDTH] * jax.nn.sigmoid(zg[..., S5_WIDTH:])


def setup_inputs(seed: int = 0) -> dict:
    key = jax.random.key(seed)
    ks = jax.random.split(key, 32)
    f32 = jnp.float32

    def nrm(k, shape, scale):
        return jax.random.normal(k, shape, f32) * scale

    def gain(k, shape):
        return 1.0 + 0.01 * jax.random.normal(k, shape, f32)

    n_idx = jnp.arange(S5_STATE, dtype=f32)
    return {
        "x": nrm(ks[0], (BATCH, SEQ, D_MODEL), 1.0),
        "p": nrm(ks[1], (DEPTH, BATCH, SEQ, PLE_DIM), 1.0),
        "rel_bias": nrm(ks[2], (REL_BUCKETS, DIFF_HEADS), 0.1),
        "norm_mix": gain(ks[3], (DEPTH, D_MODEL)),
        "w_in": nrm(ks[4], (DEPTH, D_MODEL, IN_WIDTH), D_MODEL ** -0.5),
        "diff_lambda": nrm(ks[5], (DEPTH, 4, DIFF_HEAD_DIM), 0.1),
        "diff_subln": gain(ks[6], (DEPTH, 2 * DIFF_HEAD_DIM)),
        "s5_lambda_re": -0.5 + nrm(ks[7], (DEPTH, S5_GROUPS, S5_STATE), 0.01),
        "s5_lambda_im": jnp.pi * n_idx + nrm(ks[8], (DEPTH, S5_GROUPS, S5_STATE), 0.01),
        "s5_log_dt": jax.random.uniform(ks[9], (DEPTH, S5_GROUPS), f32,
                                        minval=math.log(0.001), maxval=math.log(0.1)),
        "s5_b_re": nrm(ks[10], (DEPTH, S5_GROUPS, S5_STATE, S5_GROUP), (2 * S5_GROUP) ** -0.5),
        "s5_b_im": nrm(ks[11], (DEPTH, S5_GROUPS, S5_STATE, S5_GROUP), (2 * S5_GROUP) ** -0.5),
        "s5_c_re": nrm(ks[12], (DEPTH, S5_GROUPS, S5_GROUP, S5_STATE), (2 * S5_STATE) ** -0.5),
        "s5_c_im": nrm(ks[13], (DEPTH, S5_GROUPS, S5_GROUP, S5_STATE), (2 * S5_STATE) ** -0.5),
        "s5_d": nrm(ks[14], (DEPTH, S5_WIDTH), 1.0),
        "s5_w_glu": nrm(ks[15], (DEPTH, S5_WIDTH, 2 * S5_WIDTH), S5_WIDTH ** -0.5),
        "w_gate_up": nrm(ks[16], (DEPTH, GATE_RANK, N_BRANCHES * D_MODEL), GATE_RANK ** -0.5),
        "w_br_diff": nrm(ks[17], (DEPTH, DIFF_WIDTH, D_MODEL), DIFF_WIDTH ** -0.5),
        "w_br_ret": nrm(ks[18], (DEPTH, RET_WIDTH, D_MODEL), RET_WIDTH ** -0.5),
        "w_br_s5": nrm(ks[19], (DEPTH, S5_WIDTH, D_MODEL), S5_WIDTH ** -0.5),
        "w_o": nrm(ks[20], (DEPTH, D_MODEL, D_MODEL), D_MODEL ** -0.5),
        "norm_ffn": gain(ks[21], (DEPTH, D_MODEL)),
        "w_ffn_gate": nrm(ks[22], (DEPTH, D_MODEL, FFN_HIDDEN), D_MODEL ** -0.5),
        "w_ffn_up": nrm(ks[23], (DEPTH, D_MODEL, FFN_HIDDEN), D_MODEL ** -0.5),
        "w_ffn_down": nrm(ks[24], (DEPTH, FFN_HIDDEN, D_MODEL), FFN_HIDDEN ** -0.5),
        "norm_ple": gain(ks[25], (DEPTH, D_MODEL)),
        "w_ple": nrm(ks[26], (DEPTH, PLE_DIM, D_MODEL), PLE_DIM ** -0.5),
        "w_ple_gate_down": nrm(ks[27], (DEPTH, D_MODEL, GATE_RANK), D_MODEL ** -0.5),
        "w_ple_gate_up": nrm(ks[28], (DEPTH, GATE_RANK, D_MODEL), GATE_RANK ** -0.5),
        "norm_final": gain(ks[29], (D_MODEL,)),
    }


def reference(x, p, rel_bias, norm_mix, w_in, diff_lambda, diff_subln,
              s5_lambda_re, s5_lambda_im, s5_log_dt, s5_b_re, s5_b_im, s5_c_re, s5_c_im,
              s5_d, s5_w_glu, w_gate_up, w_br_diff, w_br_ret, w_br_s5, w_o,
              norm_ffn, w_ffn_gate, w_ffn_up, w_ffn_down,
              norm_ple, w_ple, w_ple_gate_down, w_ple_gate_up, norm_final):
    B, S = x.shape[0], x.shape[1]
    h = x
    for i in range(DEPTH):
        lam_init = 0.8 - 0.6 * math.exp(-0.3 * i)
        xn = rms_norm(h, norm_mix[i])
        z = xn @ w_in[i]
        dq = z[..., OFF_DQ:OFF_DK].reshape(B, S, DIFF_HEADS, 2, DIFF_HEAD_DIM)
        dk = z[..., OFF_DK:OFF_DV].reshape(B, S, DIFF_HEADS, 2, DIFF_HEAD_DIM)
        dv = z[..., OFF_DV:OFF_RQ].reshape(B, S, DIFF_HEADS, 2 * DIFF_HEAD_DIM)
        lq1, lk1, lq2, lk2 = (diff_lambda[i, j].astype(jnp.float32) for j in range(4))
        lam = jnp.exp(jnp.sum(lq1 * lk1)) - jnp.exp(jnp.sum(lq2 * lk2)) + lam_init
        o_diff = diff_attention(dq, dk, dv, rel_bias, lam, diff_subln[i], lam_init)
        rq = z[..., OFF_RQ:OFF_RK].reshape(B, S, RET_HEADS, RET_HEAD_DIM)
        rk = z[..., OFF_RK:OFF_RV].reshape(B, S, RET_HEADS, RET_HEAD_DIM)
        rv = z[..., OFF_RV:OFF_RG].reshape(B, S, RET_HEADS, RET_HEAD_DIM)
        o_ret = retention(rq, rk, rv, z[..., OFF_RG:OFF_SU])
        o_s5 = s5_layer(z[..., OFF_SU:OFF_GATE], s5_lambda_re[i], s5_lambda_im[i], s5_log_dt[i],
                        s5_b_re[i], s5_b_im[i], s5_c_re[i], s5_c_im[i], s5_d[i], s5_w_glu[i])
        gl = z[..., OFF_GATE:]
        wg = w_gate_up[i]
        mixed = (jax.nn.sigmoid(gl @ wg[:, 0:D_MODEL]) * (o_diff @ w_br_diff[i])
                 + jax.nn.sigmoid(gl @ wg[:, D_MODEL:2 * D_MODEL]) * (o_ret @ w_br_ret[i])
                 + jax.nn.sigmoid(gl @ wg[:, 2 * D_MODEL:]) * (o_s5 @ w_br_s5[i]))
        h = h + mixed @ w_o[i]
        hn = rms_norm(h, norm_ffn[i])
        h = h + (jax.nn.silu(hn @ w_ffn_gate[i]) * (hn @ w_ffn_up[i])) @ w_ffn_down[i]
        hp = rms_norm(h, norm_ple[i])
        gate = jax.nn.sigmoid((hp @ w_ple_gate_down[i]) @ w_ple_gate_up[i])
        h = h + (p[i] @ w_ple[i]) * gate
    return rms_norm(h, norm_final)
```

```python
import numpy as np
import concourse.bass as bass
import concourse.mybir as mybir

F32 = mybir.dt.float32
BF16 = mybir.dt.bfloat16
ALU = mybir.AluOpType
AF = mybir.ActivationFunctionType
AX = mybir.AxisListType

ENGS = ("pe", "act", "dve", "pool", "sp")


class Cell:
    __slots__ = ("name", "lw", "rd", "T")

    def __init__(self, name, T):
        self.name = name
        self.lw = None
        self.rd = {}
        self.T = T


class Tens:
    def __init__(self, prog, name, handle, ncells):
        self.p = prog
        self.name = name
        self.t = handle
        self.cells = [Cell(f"{name}.{i}", self) for i in range(ncells)]
        self.dsem = None
        self.dcount = 0

    def __getitem__(self, k):
        return self.t[k]

    @property
    def c(self):
        return self.cells[0]


class Op:
    __slots__ = ("eng", "fn", "waits", "idx", "needed", "sig", "dma", "dsem", "dval", "T")

    def __init__(self, eng, fn, idx):
        self.eng = eng
        self.fn = fn
        self.idx = idx
        self.waits = []
        self.needed = False
        self.sig = None
        self.dma = False
        self.dsem = None
        self.dval = 0
        self.T = None


class Prog:
    def __init__(self, nc):
        self.nc = nc
        self.q = {e: [] for e in ENGS}
        self.sem = {e: nc.alloc_semaphore(f"sem_{e}") for e in ENGS}
        self.waited_c = {e: {x: -1 for x in ENGS} for e in ENGS}
        self.waited_d = {e: {} for e in ENGS}
        self.tens = []
        self.n_sb = 0
        self.stack = None
        self.signum = {e: 0 for e in ENGS}
        self.cleared = False
        self.nphase = 0

    def sb(self, name, shape, dtype, ncells=1):
        if self.stack is None:
            h = self.nc.alloc_sbuf_tensor(name, list(shape), dtype)
        else:
            h = self.stack.enter_context(self.nc.sbuf_tensor(f"{name}_ph{self.nphase}", list(shape), dtype))
        T = Tens(self, name, h, ncells)
        T.scoped = self.stack is not None
        self.tens.append(T)
        return T

    def phase_begin(self):
        import contextlib
        self.stack = contextlib.ExitStack()

    def phase_end(self):
        st = self.emit()
        self.stack.close()
        self.stack = None
        self.nphase += 1
        self.q = {e: [] for e in ENGS}
        self.waited_c = {e: {x: -1 for x in ENGS} for e in ENGS}
        self.tens = [T for T in self.tens if not getattr(T, "scoped", False)]
        for T in self.tens:
            for c in T.cells:
                c.lw = None
                c.rd = {}
        return st

    def ps(self, name, shape, dtype=F32, ncells=1):
        h = self.nc.alloc_psum_tensor(name, list(shape), dtype)
        T = Tens(self, name, h, ncells)
        self.tens.append(T)
        return T

    def _dsem(self, T):
        if T.dsem is None:
            T.dsem = self.nc.alloc_semaphore(f"dsem_{T.name}")
        return T.dsem

    def _need(self, op, prod):
        if prod is None:
            return
        e = op.eng
        if prod.dma:
            T = prod.T
            full = T.dcount
            cur = self.waited_d[e].get(id(T), 0)
            if cur >= prod.dval:
                return
            self.waited_d[e][id(T)] = full
            op.waits.append(("d", T, full))
        else:
            x = prod.eng
            if x == "pe" and e == "pe":
                return
            if self.waited_c[e][x] >= prod.idx:
                return
            self.waited_c[e][x] = prod.idx
            prod.needed = True
            op.waits.append(("c", prod))

    def _track(self, op, reads, writes):
        for c in reads:
            self._need(op, c.lw)
        for c in writes:
            self._need(op, c.lw)
            for r in c.rd.values():
                if r is not op:
                    self._need(op, r)
        key = op.eng if not op.dma else ("d", id(op.T))
        for c in reads:
            c.rd[key] = op
        for c in writes:
            c.lw = op
            c.rd = {}

    @staticmethod
    def _cells(xs):
        out = []
        for x in xs:
            if hasattr(x, "cells"):
                out.extend(x.cells)
            elif isinstance(x, Cell):
                out.append(x)
            else:
                out.extend(Prog._cells(x))
        return out

    def op(self, eng, fn, reads=(), writes=()):
        o = Op(eng, fn, len(self.q[eng]))
        self._track(o, self._cells(reads), self._cells(writes))
        self.q[eng].append(o)
        return o

    def dma(self, eng, out, in_, owner, reads=(), writes=(), **kw):
        o = Op(eng, None, len(self.q[eng]))
        o.dma = True
        o.T = owner
        o.dsem = self._dsem(owner)
        self._track(o, self._cells(reads), self._cells(writes))
        owner.dcount += 16
        o.dval = owner.dcount
        o.fn = lambda E, out=out, in_=in_, kw=kw: E.dma_start(out=out, in_=in_, **kw)
        self.q[eng].append(o)
        return o

    def emit(self, final_engine="sp"):
        nc = self.nc
        for e in ENGS:
            n = self.signum[e]
            for o in self.q[e]:
                if not o.dma and o.needed:
                    n += 1
                    o.sig = n
            self.signum[e] = n
        all_sems = [self.sem[e] for e in ENGS] + [T.dsem for T in self.tens if T.dsem is not None]
        dma_T = [T for T in self.tens if T.dsem is not None]
        stats = {e: len(self.q[e]) for e in ENGS}

        def replay(e, E):
            semE = self.sem[e]
            for o in self.q[e]:
                for w in o.waits:
                    if w[0] == "c":
                        E.wait_ge(self.sem[w[1].eng], w[1].sig)
                    else:
                        E.wait_ge(w[1].dsem, w[2])
                ins = o.fn(E)
                if o.dma:
                    ins.then_inc(o.dsem, 16)
                elif o.needed:
                    ins.then_inc(semE, 1)
            if e == final_engine:
                for T in dma_T:
                    E.wait_ge(T.dsem, T.dcount)

        if not self.cleared:
            self.cleared = True
            with nc.Block() as blk0:
                @blk0.sync
                def _(E):
                    for sm in [self.sem[e] for e in ENGS]:
                        E.sem_clear(sm)
        new_d = [T for T in dma_T if not getattr(T, "dclr", False)]
        if new_d:
            with nc.Block() as blk1:
                @blk1.sync
                def _(E):
                    for T in new_d:
                        E.sem_clear(T.dsem)
                        T.dclr = True

        with nc.Block() as blk:
            @blk.tensor
            def _(E):
                replay("pe", E)

            @blk.scalar
            def _(E):
                replay("act", E)

            @blk.vector
            def _(E):
                replay("dve", E)

            @blk.gpsimd
            def _(E):
                replay("pool", E)

            @blk.sync
            def _(E):
                replay("sp", E)
        return stats

D = 4096
KC = D // 128
TT = 512
EPS = 1e-6
IN_W = 11520
FFN = 11008
NSLOT = 6


class Ctx:
    def __init__(self, p, nslot=NSLOT):
        self.p = p
        self.nslot = nslot
        self.slots = [p.sb(f"wslot{i}", [128, 4, 512], BF16) for i in range(nslot)]
        self.si = 0
        self.banks = [p.ps(f"bank{i}", [128, 512], F32) for i in range(8)]
        self.ones = p.sb("ones_bf", [128, 128], BF16)
        p.op("dve", lambda E: E.memset(self.ones[:], 1.0), writes=[self.ones])
        self.flip = 0
        self.ev = 0

    def slot(self):
        s = self.slots[self.si % self.nslot]
        self.si += 1
        return s

    def bankset(self):
        b = self.banks[0:4] if self.flip == 0 else self.banks[4:8]
        self.flip ^= 1
        return b


def wjob(cx, W, r0, nkc, c0, ncols, rhs, banks, tm=False, ntok=TT):
    p = cx.p
    nb = (ncols + 127) // 128
    kc = 0
    while kc < nkc:
        g = min(4, nkc - kc)
        s = cx.slot()
        src = W[r0 + kc * 128: r0 + (kc + g) * 128, c0:c0 + ncols].rearrange("(k p) n -> p k n", p=128)
        p.dma("pool", s[:, 0:g, 0:ncols], src, s, writes=[s])
        for k in range(g):
            a_ap, a_cells = rhs(kc + k)
            first = (kc + k == 0)
            last = (kc + k == nkc - 1)
            if not tm:
                for j in range(nb):
                    w = min(128, ncols - j * 128)
                    p.op("pe", lambda E, o=banks[j][0:w, 0:ntok], l=s[:, k, j * 128:j * 128 + w], r=a_ap, f=first, la=last:
                         E.matmul(o, l, r, start=f, stop=la), reads=[s, a_cells], writes=[banks[j]])
            else:
                for j in range(ntok // 128):
                    p.op("pe", lambda E, o=banks[j][:, 0:ncols], l=a_ap[:, j * 128:(j + 1) * 128], r=s[:, k, 0:ncols], f=first, la=last:
                         E.matmul(o, l, r, start=f, stop=la), reads=[s, a_cells], writes=[banks[j]])
        kc += g


def evac_eng(cx):
    cx.ev ^= 1
    return "act" if cx.ev else "dve"


def copy_op(p, eng, out, in_, reads, writes):
    if eng == "act":
        p.op("act", lambda E: E.copy(out, in_), reads=reads, writes=writes)
    else:
        p.op(eng, lambda E: E.tensor_copy(out, in_), reads=reads, writes=writes)


def rmsnorm_T(cx, hT, gain, outT, tmp, ntok=TT, nkc=KC, dim=D):
    p = cx.p
    ss = cx.banks[7]
    for kc in range(nkc):
        sq = tmp["sq"][kc % 2]
        p.op("act", lambda E, o=sq[:, 0:ntok], i=hT[:, kc, 0:ntok]: E.activation(o, i, AF.Square),
             reads=[hT.cells[kc]], writes=[sq])
        p.op("pe", lambda E, r=sq[:, 0:ntok], f=(kc == 0), la=(kc == nkc - 1):
             E.matmul(ss[:, 0:ntok], cx.ones[:], r, start=f, stop=la), reads=[sq, cx.ones], writes=[ss])
    rs = tmp["rs"]
    p.op("act", lambda E: E.activation(rs[:, 0:ntok], ss[:, 0:ntok], AF.Sqrt, bias=tmp["eps"][:, 0:1], scale=1.0 / dim),
         reads=[ss, tmp["eps"]], writes=[rs])
    p.op("dve", lambda E: E.reciprocal(rs[:, 0:ntok], rs[:, 0:ntok]), reads=[rs], writes=[rs])
    for kc in range(nkc):
        eng = "dve"
        p.op(eng, lambda E, o=outT[:, kc, 0:ntok], i=hT[:, kc, 0:ntok], g=gain[:, kc:kc + 1]:
             E.scalar_tensor_tensor(o, i, g, rs[:, 0:ntok], ALU.mult, ALU.mult),
             reads=[hT.cells[kc], gain, rs], writes=[outT.cells[kc]])


def norm_tmp(p):
    t = {"sq": [p.sb(f"nsq{i}", [128, TT], BF16) for i in range(2)],
         "rs": p.sb("nrs", [128, TT], F32),
         "eps": p.sb("neps", [128, 1], F32)}
    p.op("dve", lambda E: E.memset(t["eps"][:], EPS), writes=[t["eps"]])
    return t

SEG_FM = [(0, 4096, 0), (6144, 2048, 4096), (9216, 2304, 6144)]
SEG_TM = [(4096, 2048, 0), (8192, 1024, 2048)]
ZT_ROWS = 8448
ZV_COLS = 3072


def build_A(tcore):
    nc = bass.Bass("TRN2", target_bir_lowering=False)
    hT_d = nc.dram_tensor("hT", [D, tcore], F32, kind="ExternalInput").ap()
    g_d = nc.dram_tensor("g", [128, KC], F32, kind="ExternalInput").ap()
    w_d = nc.dram_tensor("w_in", [D, IN_W], F32, kind="ExternalInput").ap()
    zT_d = nc.dram_tensor("zT", [ZT_ROWS, tcore], BF16, kind="ExternalOutput").ap()
    zv_d = nc.dram_tensor("zv", [tcore, ZV_COLS], BF16, kind="ExternalOutput").ap()
    p = Prog(nc)
    cx = Ctx(p)
    tmp = norm_tmp(p)
    hT = p.sb("hT_sb", [128, KC, TT], F32, ncells=KC)
    xn = p.sb("xn_sb", [128, KC, TT], BF16, ncells=KC)
    gain = p.sb("gain", [128, KC], F32)
    stg = [p.sb(f"stg{i}", [128, 4, 512], BF16) for i in range(2)]
    p.dma("sp", gain[:], g_d, gain, writes=[gain])
    si = 0
    for t in range(tcore // TT):
        t0 = t * TT
        for q in range(4):
            src = hT_d[q * 1024:(q + 1) * 1024, t0:t0 + TT].rearrange("(k p) t -> p k t", p=128)
            p.dma("sp", hT[:, q * 8:(q + 1) * 8, :], src, hT, writes=hT.cells[q * 8:(q + 1) * 8])
        rmsnorm_T(cx, hT, gain, xn, tmp)
        rhs = lambda kc: (xn[:, kc, :], [xn.cells[kc]])
        for (c0, ncols, off) in SEG_FM:
            for n0 in range(0, ncols, 512):
                w = min(512, ncols - n0)
                banks = cx.bankset()
                wjob(cx, w_d, 0, KC, c0 + n0, w, rhs, banks)
                s = stg[si % 2]; si += 1
                nb = w // 128
                for j in range(nb):
                    copy_op(p, evac_eng(cx), s[:, j, :], banks[j][:, :], [banks[j]], [s])
                dst = zT_d[off + n0: off + n0 + w, t0:t0 + TT].rearrange("(j p) t -> p j t", p=128)
                p.dma("sp", dst, s[:, 0:nb, :], s, reads=[s])
        for (c0, ncols, off) in SEG_TM:
            for n0 in range(0, ncols, 512):
                banks = cx.bankset()
                wjob(cx, w_d, 0, KC, c0 + n0, 512, rhs, banks, tm=True)
                s = stg[si % 2]; si += 1
                for j in range(4):
                    copy_op(p, evac_eng(cx), s[:, j, :], banks[j][:, :], [banks[j]], [s])
                dst = zv_d[t0:t0 + TT, off + n0: off + n0 + 512].rearrange("(j p) n -> p j n", p=128)
                p.dma("sp", dst, s[:, :, :], s, reads=[s])
    st = p.emit()
    return nc, st

QB = 512
SCALE = 128 ** -0.5
S5L = 256


def build_B(S, parts=("diff", "ret", "s5")):
    nc = bass.Bass("TRN2", target_bir_lowering=False)
    di = lambda n, s, dt=F32: nc.dram_tensor(n, s, dt, kind="ExternalInput").ap()
    do = lambda n, s, dt=BF16: nc.dram_tensor(n, s, dt, kind="ExternalOutput").ap()
    NKB = S // 128
    dqT = di("dqT", [256, S], BF16); dkT = di("dkT", [256, S], BF16); dv = di("dv", [S, 256], BF16)
    btile = di("btile", [128, 5, 512]); cvec = di("cvec", [128, 8]); dlam = di("dlam", [128, 512]); subln = di("subln", [128, 2])
    rqT = di("rqT", [128, S], BF16); rkT = di("rkT", [128, S], BF16); rv = di("rv", [S, 128], BF16); rgT = di("rgT", [128, S], BF16)
    cosF = di("cosF", [128, S]); sinS = di("sinS", [128, S]); rconst = di("rconst", [128, 128 + 512]); ident_d = di("ident", [128, 128])
    suT = di("suT", [128, S], BF16)
    s5bj = di("s5bj", [128, 64 * 4 + 1]); bmask = di("bmask", [128, 512]); s5st = di("s5st", [128, 12]); cblk = di("cblk", [128, 8, 128])
    s5d = di("s5d", [128, 1]); iota = di("iota", [128, S5L + 1])
    odT = do("odT", [256, S]); orT = do("orT", [128, S]); ysT = do("ysT", [128, S])

    p = Prog(nc)
    cx = Ctx(p, nslot=0)
    bk = cx.banks
    cv = p.sb("cvec_sb", [128, 8], F32)
    p.dma("sp", cv[:], cvec, cv, writes=[cv])
    ident = p.sb("ident_f", [128, 128], F32); identb = p.sb("ident_b", [128, 128], BF16)
    p.dma("sp", ident[:], ident_d, ident, writes=[ident])
    p.op("dve", lambda E: E.tensor_copy(identb[:], ident[:]), reads=[ident], writes=[identb])

    def vop(eng, fn, reads, writes):
        p.op(eng, fn, reads=reads, writes=writes)

    def tt_op(eng, o, a, b, op, reads, writes):
        p.op(eng, lambda E: E.tensor_tensor(o, a, b, op), reads=reads, writes=writes)

    if "diff" in parts:
        p.phase_begin()
        kT = [p.sb(f"kT{m}", [128, S], BF16) for m in range(2)]
        vv = p.sb("vv", [128, NKB, 256], BF16)
        bt = p.sb("bt", [128, 5, 512], F32)
        lamt = p.sb("lamt", [128, 512], F32); lw = p.sb("lw", [128, 256], F32); ls = p.sb("ls", [128, 4], F32)
        sl = p.sb("subln_sb", [128, 2], F32)
        qt = [[p.sb(f"qt{b}{m}", [128, QB], BF16) for m in range(2)] for b in range(2)]
        pt = [p.sb(f"pt{i}", [128, QB], BF16) for i in range(3)]
        nt = [p.sb(f"nt{i}", [128, QB], F32) for i in range(2)]
        r = [p.sb(f"rr{i}", [128, QB], F32) for i in range(2)]
        oa = [p.sb(f"oa{i}", [128, QB], F32) for i in range(2)]
        ob = p.sb("ob", [128, QB], F32)
        sqd = [p.sb(f"sqd{i}", [128, QB], BF16) for i in range(2)]
        rsd = p.sb("rsd", [128, QB], F32)
        ost = [p.sb(f"ost{i}", [128, 2, QB], BF16) for i in range(2)]
        for m in range(2):
            for hh in range(0, S, 4096):
                w = min(4096, S - hh)
                p.dma("sp", kT[m][:, hh:hh + w], dkT[m * 128:(m + 1) * 128, hh:hh + w], kT[m], writes=[kT[m]])
        for hh in range(0, NKB, 32):
            w = min(32, NKB - hh)
            p.dma("sp", vv[:, hh:hh + w, :], dv[hh * 128:(hh + w) * 128, :].rearrange("(b p) e -> p b e", p=128), vv, writes=[vv])
        p.dma("sp", bt[:], btile, bt, writes=[bt])
        p.dma("sp", lamt[:], dlam, lamt, writes=[lamt])
        p.dma("sp", sl[:], subln, sl, writes=[sl])
        tt_op("dve", lw[:, 0:128], lamt[:, 0:128], lamt[:, 128:256], ALU.mult, [lamt], [lw])
        tt_op("dve", lw[:, 128:256], lamt[:, 256:384], lamt[:, 384:512], ALU.mult, [lamt], [lw])
        vop("dve", lambda E: E.reduce_sum(ls[:, 0:1], lw[:, 0:128], AX.X), [lw], [ls])
        vop("dve", lambda E: E.reduce_sum(ls[:, 1:2], lw[:, 128:256], AX.X), [lw], [ls])
        vop("act", lambda E: E.activation(ls[:, 0:2], ls[:, 0:2], AF.Exp), [ls], [ls])
        tt_op("dve", ls[:, 2:3], ls[:, 1:2], ls[:, 0:1], ALU.subtract, [ls], [ls])
        tt_op("dve", ls[:, 2:3], ls[:, 2:3], cv[:, 1:2], ALU.subtract, [ls, cv], [ls])
        vop("dve", lambda E: E.tensor_scalar(sl[:], sl[:], cv[:, 2:3], None, ALU.mult), [sl, cv], [sl])
        pti = 0
        for qb in range(S // QB):
            q0 = qb * QB
            qq = qt[qb % 2]
            for m in range(2):
                p.dma("sp", qq[m][:], dqT[m * 128:(m + 1) * 128, q0:q0 + QB], qq[m], writes=[qq[m]])
            nkb = q0 // 128 + 4
            for m in range(2):
                accs = (bk[3 * m], bk[3 * m + 1], bk[3 * m + 2])
                for kb in range(nkb):
                    j = kb - q0 // 128
                    qlo = max(0, 128 * j)
                    sc = bk[6 + (kb % 2)]
                    vop("pe", lambda E, o=sc[:, qlo:QB], l=kT[m][:, kb * 128:(kb + 1) * 128], rr=qq[m][:, qlo:QB]:
                        E.matmul(o, l, rr, start=True, stop=True), [kT[m], qq[m]], [sc])
                    P = pt[pti % 3]; pti += 1
                    if j < -1:
                        vop("act", lambda E, o=P[:, qlo:QB], i=sc[:, qlo:QB]:
                            E.activation(o, i, AF.Exp, bias=cv[:, 0:1], scale=SCALE), [sc, cv], [P])
                    else:
                        N = nt[kb % 2]
                        vop("dve", lambda E, o=N[:, qlo:QB], i=sc[:, qlo:QB], b=bt[:, j + 1, qlo:QB]:
                            E.scalar_tensor_tensor(o, i, SCALE, b, ALU.mult, ALU.add), [sc, bt], [N])
                        vop("act", lambda E, o=P[:, qlo:QB], i=N[:, qlo:QB]: E.activation(o, i, AF.Exp), [N], [P])
                    f, la = (kb == 0), (kb == nkb - 1)
                    for es in range(2):
                        vop("pe", lambda E, o=accs[es][:, qlo:QB], l=vv[:, kb, es * 128:(es + 1) * 128], rr=P[:, qlo:QB], f=f, la=la:
                            E.matmul(o, l, rr, start=f, stop=la), [vv, P], [accs[es]])
                    vop("pe", lambda E, o=accs[2][:, qlo:QB], rr=P[:, qlo:QB], f=f, la=la:
                        E.matmul(o, cx.ones[:], rr, start=f, stop=la), [cx.ones, P], [accs[2]])
            vop("dve", lambda E: E.reciprocal(r[0][:], bk[2][:, :]), [bk[2]], [r[0]])
            vop("dve", lambda E: E.reciprocal(r[1][:], bk[5][:, :]), [bk[5]], [r[1]])
            vop("dve", lambda E: E.tensor_scalar(r[1][:], r[1][:], ls[:, 2:3], None, ALU.mult), [r[1], ls], [r[1]])
            for es in range(2):
                tt_op("dve", oa[es][:], bk[es][:, :], r[0][:], ALU.mult, [bk[es], r[0]], [oa[es]])
                tt_op("dve", ob[:], bk[3 + es][:, :], r[1][:], ALU.mult, [bk[3 + es], r[1]], [ob])
                tt_op("pool", oa[es][:], oa[es][:], ob[:], ALU.add, [oa[es], ob], [oa[es]])
                vop("act", lambda E, o=sqd[es][:], i=oa[es][:]: E.activation(o, i, AF.Square), [oa[es]], [sqd[es]])
                vop("pe", lambda E, rr=sqd[es][:], f=(es == 0), la=(es == 1):
                    E.matmul(bk[6][:, :], cx.ones[:], rr, start=f, stop=la), [cx.ones, sqd[es]], [bk[6]])
            vop("act", lambda E: E.activation(rsd[:], bk[6][:, :], AF.Sqrt, bias=cv[:, 3:4], scale=1.0 / 256), [bk[6], cv], [rsd])
            vop("dve", lambda E: E.reciprocal(rsd[:], rsd[:]), [rsd], [rsd])
            O = ost[qb % 2]
            for es in range(2):
                vop("dve", lambda E, o=O[:, es, :], i=oa[es][:], g=sl[:, es:es + 1]:
                    E.scalar_tensor_tensor(o, i, g, rsd[:], ALU.mult, ALU.mult), [oa[es], sl, rsd], [O])
            p.dma("sp", odT[:, q0:q0 + QB].rearrange("(e p) t -> p e t", p=128), O[:, :, :], O, reads=[O])
        p.phase_end()

    if "ret" in parts:
        p.phase_begin()
        rc = p.sb("rconst_sb", [128, 128 + 512], F32)
        p.dma("sp", rc[:], rconst, rc, writes=[rc])
        NB = 2
        rq = [p.sb(f"rq{i}", [128, 512], BF16) for i in range(NB)]; rqs = [p.sb(f"rqs{i}", [128, 512], BF16) for i in range(NB)]
        rk = [p.sb(f"rk{i}", [128, 512], BF16) for i in range(NB)]; rks = [p.sb(f"rks{i}", [128, 512], BF16) for i in range(NB)]
        rg = [p.sb(f"rg{i}", [128, 512], BF16) for i in range(NB)]
        rvv = [p.sb(f"rvv{i}", [128, 4, 128], BF16) for i in range(NB)]
        cs = [p.sb(f"cs{i}", [128, 512], F32) for i in range(NB)]; sn = [p.sb(f"sn{i}", [128, 512], F32) for i in range(NB)]
        ta = p.sb("r_ta", [128, 512], F32); tb = p.sb("r_tb", [128, 512], F32)
        qr = p.sb("r_qr", [128, 512], BF16); kr = p.sb("r_kr", [128, 512], BF16); qx = p.sb("r_qx", [128, 512], BF16)
        kz = [p.sb(f"r_kz{i}", [128, 128], BF16) for i in range(2)]
        inT = [p.sb(f"r_inT{i}", [128, 128], BF16) for i in range(2)]
        R = p.sb("r_R", [128, 128], F32); Rb = [p.sb(f"r_Rb{i}", [128, 128], BF16) for i in range(2)]
        sqr = p.sb("r_sq", [128, 512], BF16); rsr = p.sb("r_rs", [128, 512], F32); sgr = p.sb("r_sg", [128, 512], F32)
        orr = p.sb("r_or", [128, 512], F32); oro = [p.sb(f"r_oo{i}", [128, 512], BF16) for i in range(2)]
        vop("dve", lambda E: E.memset(R[:], 0.0), [], [R])
        nch = 0
        for sc_i in range(S // 512):
            t0 = sc_i * 512
            b = sc_i % NB
            cols = slice(t0, t0 + 512)
            p.dma("sp", rq[b][:], rqT[:, cols], rq[b], writes=[rq[b]])
            p.dma("sp", rqs[b][0:64, :], rqT[64:128, cols], rqs[b], writes=[rqs[b]])
            p.dma("sp", rqs[b][64:128, :], rqT[0:64, cols], rqs[b], writes=[rqs[b]])
            p.dma("sp", rk[b][:], rkT[:, cols], rk[b], writes=[rk[b]])
            p.dma("sp", rks[b][0:64, :], rkT[64:128, cols], rks[b], writes=[rks[b]])
            p.dma("sp", rks[b][64:128, :], rkT[0:64, cols], rks[b], writes=[rks[b]])
            p.dma("sp", rg[b][:], rgT[:, cols], rg[b], writes=[rg[b]])
            p.dma("sp", rvv[b][:], rv[cols, :].rearrange("(c p) e -> p c e", p=128), rvv[b], writes=[rvv[b]])
            p.dma("sp", cs[b][:], cosF[:, cols], cs[b], writes=[cs[b]])
            p.dma("sp", sn[b][:], sinS[:, cols], sn[b], writes=[sn[b]])
            tt_op("dve", ta[:], rq[b][:], cs[b][:], ALU.mult, [rq[b], cs[b]], [ta])
            tt_op("pool", tb[:], rqs[b][:], sn[b][:], ALU.mult, [rqs[b], sn[b]], [tb])
            tt_op("dve", qr[:], ta[:], tb[:], ALU.add, [ta, tb], [qr])
            tt_op("pool", qx[:], qr[:], rc[:, 128:640], ALU.mult, [qr, rc], [qx])
            tt_op("dve", ta[:], rk[b][:], cs[b][:], ALU.mult, [rk[b], cs[b]], [ta])
            tt_op("pool", tb[:], rks[b][:], sn[b][:], ALU.mult, [rks[b], sn[b]], [tb])
            tt_op("dve", kr[:], ta[:], tb[:], ALU.add, [ta, tb], [kr])
            ops = bk[sc_i % 2]
            for ch in range(4):
                cc = slice(ch * 128, (ch + 1) * 128)
                ib = bk[2 + (nch % 2)]
                vop("pe", lambda E, o=ib[:, 0:128], l=kr[:, cc], rr=qr[:, cc]: E.matmul(o, l, rr, start=True, stop=True), [kr, qr], [ib])
                I = inT[nch % 2]
                tt_op("dve", I[:], ib[:, 0:128], rc[:, 0:128], ALU.mult, [ib, rc], [I])
                first = (nch == 0)
                vop("pe", lambda E, o=ops[:, cc], l=rvv[b][:, ch, :], rr=I[:], la=first: E.matmul(o, l, rr, start=True, stop=la),
                    [rvv[b], I], [ops])
                if not first:
                    vop("pe", lambda E, o=ops[:, cc], l=Rb[nch % 2][:], rr=qx[:, cc]: E.matmul(o, l, rr, start=False, stop=True),
                        [Rb[nch % 2], qx], [ops])
                vop("pe", lambda E, l=kr[:, cc]: E.matmul(bk[5][:, 0:128], l, identb[:], start=True, stop=True), [kr, identb], [bk[5]])
                KZ = kz[nch % 2]
                vop("dve", lambda E, o=KZ[:]: E.tensor_scalar(o, bk[5][:, 0:128], cv[:, 4:5], None, ALU.mult), [bk[5], cv], [KZ])
                vop("pe", lambda E, l=KZ[:], rr=rvv[b][:, ch, :]: E.matmul(bk[4][:, 0:128], l, rr, start=True, stop=True), [KZ, rvv[b]], [bk[4]])
                vop("dve", lambda E: E.scalar_tensor_tensor(R[:], R[:], cv[:, 5:6], bk[4][:, 0:128], ALU.mult, ALU.add), [R, cv, bk[4]], [R])
                nch += 1
                vop("pool", lambda E, o=Rb[nch % 2][:]: E.tensor_copy(o, R[:]), [R], [Rb[nch % 2]])
            vop("act", lambda E, ops=ops: E.activation(sqr[:], ops[:, :], AF.Square), [ops], [sqr])
            vop("pe", lambda E: E.matmul(bk[6][:, :], cx.ones[:], sqr[:], start=True, stop=True), [cx.ones, sqr], [bk[6]])
            vop("act", lambda E: E.activation(rsr[:], bk[6][:, :], AF.Sqrt, bias=cv[:, 3:4], scale=1.0 / 128), [bk[6], cv], [rsr])
            vop("dve", lambda E: E.reciprocal(rsr[:], rsr[:]), [rsr], [rsr])
            vop("act", lambda E, g_=rg[b]: E.activation(sgr[:], g_[:], AF.Silu), [rg[b]], [sgr])
            tt_op("dve", orr[:], ops[:, :], rsr[:], ALU.mult, [ops, rsr], [orr])
            OO = oro[sc_i % 2]
            tt_op("pool", OO[:], orr[:], sgr[:], ALU.mult, [orr, sgr], [OO])
            p.dma("sp", orT[:, cols], OO[:], OO, reads=[OO])
        p.phase_end()

    if "s5" in parts:
        p.phase_begin()
        L = S5L
        bj = p.sb("s5bj_sb", [128, 257], F32)
        st = p.sb("s5st_sb", [128, 12], F32)
        bm = p.sb("bmask_sb", [128, 512], F32)
        cb = p.sb("cblk_f", [128, 8, 128], F32); cbb = p.sb("cblk_b", [128, 8, 128], BF16)
        dsk = p.sb("s5d_sb", [128, 1], F32); io = p.sb("iota_sb", [128, L + 1], F32)
        for (T_, src) in ((bj, s5bj), (st, s5st), (bm, bmask), (cb, cblk), (dsk, s5d), (io, iota)):
            p.dma("sp", T_[:], src, T_, writes=[T_])
        vop("pool", lambda E: E.tensor_copy(cbb[:], cb[:]), [cb], [cbb])
        TWO_PI = 2.0 * np.pi

        MAGIC = 12582912.0

        def frac(dst, src, scr, shape):
            vop("dve", lambda E: E.tensor_scalar(scr[:], src[:], MAGIC, None, ALU.add), [src], [scr])
            vop("dve", lambda E: E.tensor_scalar(scr[:], scr[:], -MAGIC, None, ALU.add), [scr], [scr])
            tt_op("dve", dst[:], src[:], scr[:], ALU.subtract, [src, scr], [dst])

        def disc(n, lr, li, ldt, pre, src, scalar_dt=False):
            o = {k: p.sb(f"{pre}_{k}", [128, n], F32) for k in ("dt", "mag", "th", "ar", "ai", "t1", "t2")}
            ndt = 1 if scalar_dt else n
            dt_ = p.sb(f"{pre}_dtt", [128, ndt], F32)
            vop("act", lambda E: E.activation(dt_[:], ldt, AF.Exp), [src], [dt_])
            if scalar_dt:
                vop("dve", lambda E: E.tensor_scalar(o["mag"][:], lr, dt_[:, 0:1], None, ALU.mult), [dt_, src], [o["mag"]])
            else:
                tt_op("dve", o["mag"][:], lr, dt_[:], ALU.mult, [dt_, src], [o["mag"]])
            vop("act", lambda E: E.activation(o["mag"][:], o["mag"][:], AF.Exp), [o["mag"]], [o["mag"]])
            if scalar_dt:
                vop("dve", lambda E: E.tensor_scalar(o["th"][:], li, dt_[:, 0:1], None, ALU.mult), [dt_, src], [o["th"]])
            else:
                tt_op("dve", o["th"][:], li, dt_[:], ALU.mult, [dt_, src], [o["th"]])
            vop("dve", lambda E: E.tensor_scalar(o["th"][:], o["th"][:], 1.0 / TWO_PI, None, ALU.mult), [o["th"]], [o["th"]])
            frac(o["th"], o["th"], o["t1"], [128, n])
            vop("act", lambda E: E.activation(o["t1"][:], o["th"][:], AF.Sin, scale=TWO_PI), [o["th"]], [o["t1"]])
            tt_op("dve", o["ai"][:], o["t1"][:], o["mag"][:], ALU.mult, [o["t1"], o["mag"]], [o["ai"]])
            vop("dve", lambda E: E.tensor_scalar(o["t2"][:], o["th"][:], 0.25, None, ALU.add), [o["th"]], [o["t2"]])
            frac(o["t2"], o["t2"], o["t1"], [128, n])
            vop("act", lambda E: E.activation(o["t2"][:], o["t2"][:], AF.Sin, scale=TWO_PI), [o["t2"]], [o["t2"]])
            tt_op("dve", o["ar"][:], o["t2"][:], o["mag"][:], ALU.mult, [o["t2"], o["mag"]], [o["ar"]])
            return o
        dj = disc(64, bj[:, 0:64], bj[:, 64:128], bj[:, 256:257], "dj", bj, scalar_dt=True)
        den = p.sb("dj_den", [128, 64], F32); fr = p.sb("dj_fr", [128, 64], F32); fi = p.sb("dj_fi", [128, 64], F32)
        am1 = p.sb("dj_am1", [128, 64], F32); tq = p.sb("dj_tq", [128, 64], F32)
        lr, li = bj[:, 0:64], bj[:, 64:128]
        tt_op("dve", den[:], lr, lr, ALU.mult, [bj], [den])
        tt_op("dve", tq[:], li, li, ALU.mult, [bj], [tq])
        tt_op("dve", den[:], den[:], tq[:], ALU.add, [den, tq], [den])
        vop("dve", lambda E: E.reciprocal(den[:], den[:]), [den], [den])
        vop("dve", lambda E: E.tensor_scalar(am1[:], dj["ar"][:], -1.0, None, ALU.add), [dj["ar"]], [am1])
        tt_op("dve", fr[:], am1[:], lr, ALU.mult, [am1, bj], [fr])
        tt_op("dve", tq[:], dj["ai"][:], li, ALU.mult, [dj["ai"], bj], [tq])
        tt_op("dve", fr[:], fr[:], tq[:], ALU.add, [fr, tq], [fr])
        tt_op("dve", fr[:], fr[:], den[:], ALU.mult, [fr, den], [fr])
        tt_op("dve", fi[:], dj["ai"][:], lr, ALU.mult, [dj["ai"], bj], [fi])
        tt_op("dve", tq[:], am1[:], li, ALU.mult, [am1, bj], [tq])
        tt_op("dve", fi[:], fi[:], tq[:], ALU.subtract, [fi, tq], [fi])
        tt_op("dve", fi[:], fi[:], den[:], ALU.mult, [fi, den], [fi])
        bbr = p.sb("bbr", [128, 64], F32); bbi = p.sb("bbi", [128, 64], F32)
        bre, bim = bj[:, 128:192], bj[:, 192:256]
        tt_op("dve", bbr[:], fr[:], bre, ALU.mult, [fr, bj], [bbr])
        tt_op("dve", tq[:], fi[:], bim, ALU.mult, [fi, bj], [tq])
        tt_op("dve", bbr[:], bbr[:], tq[:], ALU.subtract, [bbr, tq], [bbr])
        tt_op("dve", bbi[:], fr[:], bim, ALU.mult, [fr, bj], [bbi])
        tt_op("dve", tq[:], fi[:], bre, ALU.mult, [fi, bj], [tq])
        tt_op("dve", bbi[:], bbi[:], tq[:], ALU.add, [bbi, tq], [bbi])
        Bre = p.sb("Bre", [128, 512], BF16); Bim = p.sb("Bim", [128, 512], BF16)
        for g_ in range(8):
            gs = slice(g_ * 64, (g_ + 1) * 64)
            tt_op("dve", Bre[:, gs], bm[:, gs], bbr[:], ALU.mult, [bm, bbr], [Bre])
            tt_op("dve", Bim[:, gs], bm[:, gs], bbi[:], ALU.mult, [bm, bbi], [Bim])
        ds = disc(4, st[:, 0:4], st[:, 4:8], st[:, 8:12], "ds", st)
        cosT = p.sb("cosT", [128, 4, L + 1], F32); sinT = p.sb("sinT", [128, 4, L + 1], F32); mt = p.sb("mt", [128, 4, L], F32)
        ang = p.sb("ang", [128, L + 1], F32); ang2 = p.sb("ang2", [128, L + 1], F32); ang3 = p.sb("ang3", [128, L + 1], F32)
        for s_ in range(4):
            th = ds["th"][:, s_:s_ + 1]
            vop("dve", lambda E, th=th: E.tensor_scalar(ang[:], io[:], th, None, ALU.mult), [io, ds["th"]], [ang])
            frac(ang2, ang, ang3, None)
            vop("act", lambda E, s_=s_: E.activation(sinT[:, s_, :], ang2[:], AF.Sin, scale=TWO_PI), [ang2], [sinT])
            vop("dve", lambda E: E.tensor_scalar(ang[:], ang[:], 0.25, None, ALU.add), [ang], [ang])
            frac(ang2, ang, ang3, None)
            vop("act", lambda E, s_=s_: E.activation(cosT[:, s_, :], ang2[:], AF.Sin, scale=TWO_PI), [ang2], [cosT])
            vop("dve", lambda E, s_=s_: E.memset(mt[:, s_, :], 1.0), [], [mt])
            vop("dve", lambda E, s_=s_: E.tensor_scalar(mt[:, s_, :], mt[:, s_, :], ds["mag"][:, s_:s_ + 1], None, ALU.mult), [mt, ds["mag"]], [mt])
        ub = [p.sb(f"s5u{i}", [128, L], BF16) for i in range(2)]
        wt_ = {k: p.sb(f"s5_{k}", [128, L], F32) for k in ("t1", "t2", "t3", "t4", "wr", "wi", "gr", "gi", "u1", "u2", "u3", "u4", "y", "x2", "sg")}
        hrb = [p.sb(f"s5hr{i}", [128, L], BF16) for i in range(2)]; hib = [p.sb(f"s5hi{i}", [128, L], BF16) for i in range(2)]
        ini = p.sb("s5ini", [128, 4, 2], F32); tini = p.sb("s5tini", [128, 1], F32)
        yo = [p.sb(f"s5yo{i}", [128, L], BF16) for i in range(2)]
        vop("dve", lambda E: E.memset(ini[:], 0.0), [], [ini])
        W = wt_
        for c_ in range(S // L):
            cols = slice(c_ * L, (c_ + 1) * L)
            U = ub[c_ % 2]
            p.dma("sp", U[:], suT[:, cols], U, writes=[U])
            yps = bk[4 + (c_ % 2)]
            for s_ in range(4):
                xb = bk[s_]
                vop("pe", lambda E, s_=s_, xb=xb, U=U: E.matmul(xb[:, 0:L], Bre[:, s_ * 128:(s_ + 1) * 128], U[:], start=True, stop=True), [Bre, U], [xb])
                vop("pe", lambda E, s_=s_, xb=xb, U=U: E.matmul(xb[:, L:2 * L], Bim[:, s_ * 128:(s_ + 1) * 128], U[:], start=True, stop=True), [Bim, U], [xb])
                co, si = cosT[:, s_, 0:L], sinT[:, s_, 0:L]
                xr, xi_ = xb[:, 0:L], xb[:, L:2 * L]
                tt_op("dve", W["t1"][:], xr, co, ALU.mult, [xb, cosT], [W["t1"]])
                tt_op("dve", W["t2"][:], xi_, si, ALU.mult, [xb, sinT], [W["t2"]])
                tt_op("pool", W["wr"][:], W["t1"][:], W["t2"][:], ALU.add, [W["t1"], W["t2"]], [W["wr"]])
                tt_op("dve", W["t3"][:], xi_, co, ALU.mult, [xb, cosT], [W["t3"]])
                tt_op("dve", W["t4"][:], xr, si, ALU.mult, [xb, sinT], [W["t4"]])
                tt_op("pool", W["wi"][:], W["t3"][:], W["t4"][:], ALU.subtract, [W["t3"], W["t4"]], [W["wi"]])
                vop("dve", lambda E, s_=s_: E.tensor_tensor_scan(W["gr"][:], mt[:, s_, :], W["wr"][:], ini[:, s_, 0:1], ALU.mult, ALU.add),
                    [mt, W["wr"], ini], [W["gr"]])
                vop("dve", lambda E, s_=s_: E.tensor_tensor_scan(W["gi"][:], mt[:, s_, :], W["wi"][:], ini[:, s_, 1:2], ALU.mult, ALU.add),
                    [mt, W["wi"], ini], [W["gi"]])
                cL, sL = cosT[:, s_, L:L + 1], sinT[:, s_, L:L + 1]
                grl, gil = W["gr"][:, L - 1:L], W["gi"][:, L - 1:L]
                vop("dve", lambda E, gil=gil, sL=sL: E.tensor_scalar(tini[:], gil, sL, None, ALU.mult), [W["gi"], sinT], [tini])
                vop("dve", lambda E, s_=s_, grl=grl, cL=cL: E.scalar_tensor_tensor(ini[:, s_, 0:1], grl, cL, tini[:], ALU.mult, ALU.subtract),
                    [W["gr"], cosT, tini], [ini])
                vop("dve", lambda E, grl=grl, sL=sL: E.tensor_scalar(tini[:], grl, sL, None, ALU.mult), [W["gr"], sinT], [tini])
                vop("dve", lambda E, s_=s_, gil=gil, cL=cL: E.scalar_tensor_tensor(ini[:, s_, 1:2], gil, cL, tini[:], ALU.mult, ALU.add),
                    [W["gi"], cosT, tini], [ini])
                HR, HI = hrb[s_ % 2], hib[s_ % 2]
                tt_op("pool", W["u1"][:], W["gr"][:], co, ALU.mult, [W["gr"], cosT], [W["u1"]])
                tt_op("pool", W["u2"][:], W["gi"][:], si, ALU.mult, [W["gi"], sinT], [W["u2"]])
                tt_op("pool", HR[:], W["u1"][:], W["u2"][:], ALU.subtract, [W["u1"], W["u2"]], [HR])
                tt_op("pool", W["u3"][:], W["gr"][:], si, ALU.mult, [W["gr"], sinT], [W["u3"]])
                tt_op("pool", W["u4"][:], W["gi"][:], co, ALU.mult, [W["gi"], cosT], [W["u4"]])
                vop("dve", lambda E, HI=HI: E.scalar_tensor_tensor(HI[:], W["u3"][:], -1.0, W["u4"][:], ALU.mult, ALU.subtract),
                    [W["u3"], W["u4"]], [HI])
                vop("pe", lambda E, s_=s_, HR=HR, yps=yps: E.matmul(yps[:, 0:L], cbb[:, 2 * s_, :], HR[:], start=(s_ == 0), stop=False), [cbb, HR], [yps])
                vop("pe", lambda E, s_=s_, HI=HI, yps=yps: E.matmul(yps[:, 0:L], cbb[:, 2 * s_ + 1, :], HI[:], start=False, stop=(s_ == 3)), [cbb, HI], [yps])
            vop("dve", lambda E, U=U, yps=yps: E.scalar_tensor_tensor(W["y"][:], U[:], dsk[:, 0:1], yps[:, 0:L], ALU.mult, ALU.add), [U, dsk, yps], [W["y"]])
            tt_op("pool", W["x2"][:], W["y"][:], W["y"][:], ALU.mult, [W["y"]], [W["x2"]])
            vop("pool", lambda E: E.tensor_scalar(W["x2"][:], W["x2"][:], 0.044715, 1.0, ALU.mult, ALU.add), [W["x2"]], [W["x2"]])
            tt_op("pool", W["x2"][:], W["x2"][:], W["y"][:], ALU.mult, [W["x2"], W["y"]], [W["x2"]])
            vop("act", lambda E: E.activation(W["sg"][:], W["x2"][:], AF.Sigmoid, scale=2.0 * float(np.sqrt(2.0 / np.pi))), [W["x2"]], [W["sg"]])
            YO = yo[c_ % 2]
            tt_op("pool", YO[:], W["y"][:], W["sg"][:], ALU.mult, [W["y"], W["sg"]], [YO])
            p.dma("sp", ysT[:, cols], YO[:], YO, reads=[YO])
        p.phase_end()
    if any(p.q[e] for e in ENGS):
        p.emit()
    return nc, None

FH = FFN // 2
FHB = FH // 128


def build_C(tcore, final):
    nc = bass.Bass("TRN2", target_bir_lowering=False)
    di = lambda n, s, dt=F32: nc.dram_tensor(n, s, dt, kind="ExternalInput").ap()
    hT_d = di("hT", [D, tcore])
    glT_d = di("glT", [256, tcore], BF16)
    odT_d = di("odT", [2048, tcore], BF16)
    orT_d = di("orT", [1024, tcore], BF16)
    ysT_d = di("ysT", [1024, tcore], BF16)
    pT_d = di("pT", [256, tcore])
    gains_d = di("gains", [128, 3 * KC])
    w_glu = di("w_glu", [1024, 2048]); w_gu = di("w_gu", [256, 3 * D])
    w_bd = di("w_bd", [2048, D]); w_br = di("w_br", [1024, D]); w_bs = di("w_bs", [1024, D])
    w_o = di("w_o", [D, D]); w_fg = di("w_fg", [D, FFN]); w_fu = di("w_fu", [D, FFN]); w_fd = di("w_fd", [FFN, D])
    w_ple = di("w_ple", [256, D]); w_pgd = di("w_pgd", [D, 256]); w_pgu = di("w_pgu", [256, D])
    outT_d = nc.dram_tensor("outT", [D, tcore], F32, kind="ExternalOutput").ap()

    p = Prog(nc)
    cx = Ctx(p)
    tmp = norm_tmp(p)
    hT = p.sb("hT_sb", [128, KC, TT], F32, ncells=KC)
    big = p.sb("big", [128, FHB, TT], BF16, ncells=FHB)
    x32 = p.sb("x32", [128, KC, TT], BF16, ncells=KC)
    glT = p.sb("glT_sb", [128, 2, TT], BF16, ncells=2)
    pT = p.sb("pT_sb", [128, 2, TT], BF16, ncells=2)
    gdT = p.sb("gdT_sb", [128, 2, TT], BF16, ncells=2)
    gains = p.sb("gains_sb", [128, 3 * KC], F32)
    sg = [p.sb(f"sg{i}", [128, TT], F32) for i in range(4)]
    tt = [p.sb(f"tt{i}", [128, TT], F32) for i in range(2)]
    macc = [p.sb(f"macc{i}", [128, TT], F32) for i in range(4)]
    p.dma("sp", gains[:], gains_d, gains, writes=[gains])
    g_ffn = gains[:, 0:KC]; g_ple = gains[:, KC:2 * KC]; g_fin = gains[:, 2 * KC:3 * KC]
    tti = [0]

    def nexttt():
        tti[0] += 1
        return tt[tti[0] % 2]

    def bigrhs(base):
        return lambda kc: (big[:, base + kc, :], [big.cells[base + kc]])

    def act_to(func, dst, src):
        p.op("act", lambda E: E.activation(dst[:], src[:, :], func), reads=[src], writes=[dst])

    for t in range(tcore // TT):
        t0 = t * TT
        cols = slice(t0, t0 + TT)
        for q in range(4):
            src = hT_d[q * 1024:(q + 1) * 1024, cols].rearrange("(k p) t -> p k t", p=128)
            p.dma("sp", hT[:, q * 8:(q + 1) * 8, :], src, hT, writes=hT.cells[q * 8:(q + 1) * 8])
        for (src_d, base, n) in ((odT_d, 0, 16), (orT_d, 16, 8), (ysT_d, 24, 8)):
            for q in range(0, n, 8):
                src = src_d[q * 128:(q + 8) * 128, cols].rearrange("(k p) t -> p k t", p=128)
                p.dma("sp", big[:, base + q:base + q + 8, :], src, big, writes=big.cells[base + q:base + q + 8])
        p.dma("sp", glT[:, :, :], glT_d[:, cols].rearrange("(k p) t -> p k t", p=128), glT, writes=[glT])
        p.dma("pool", pT[:, :, :], pT_d[:, cols].rearrange("(k p) t -> p k t", p=128), pT, writes=[pT])

        for n0 in (0, 512):
            A = cx.bankset(); wjob(cx, w_glu, 0, 8, n0, 512, bigrhs(24), A)
            B = cx.bankset(); wjob(cx, w_glu, 0, 8, 1024 + n0, 512, bigrhs(24), B)
            for j in range(4):
                act_to(AF.Sigmoid, sg[j], B[j])
                c = 32 + n0 // 128 + j
                p.op("dve", lambda E, o=big[:, c, :], a=A[j][:, :], s=sg[j][:]: E.tensor_tensor(o, a, s, ALU.mult),
                     reads=[A[j], sg[j]], writes=[big.cells[c]])

        for n0 in range(0, D, 512):
            for b, (Wb, nk, base) in enumerate(((w_bd, 16, 0), (w_br, 8, 16), (w_bs, 8, 32))):
                G = cx.bankset()
                wjob(cx, w_gu, 0, 2, b * D + n0, 512, lambda kc: (glT[:, kc, :], [glT.cells[kc]]), G)
                B = cx.bankset()
                wjob(cx, Wb, 0, nk, n0, 512, bigrhs(base), B)
                for j in range(4):
                    act_to(AF.Sigmoid, sg[j], G[j])
                    c = n0 // 128 + j
                    if b == 0:
                        p.op("dve", lambda E, o=macc[j][:], a=B[j][:, :], s=sg[j][:]: E.tensor_tensor(o, a, s, ALU.mult),
                             reads=[B[j], sg[j]], writes=[macc[j]])
                    else:
                        x = nexttt()
                        p.op("dve", lambda E, o=x[:], a=B[j][:, :], s=sg[j][:]: E.tensor_tensor(o, a, s, ALU.mult),
                             reads=[B[j], sg[j]], writes=[x])
                        if b == 1:
                            p.op("pool", lambda E, o=macc[j][:], a=macc[j][:], s=x[:]: E.tensor_tensor(o, a, s, ALU.add),
                                 reads=[macc[j], x], writes=[macc[j]])
                        else:
                            p.op("pool", lambda E, o=x32[:, c, :], a=macc[j][:], s=x[:]: E.tensor_tensor(o, a, s, ALU.add),
                                 reads=[macc[j], x], writes=[x32.cells[c]])

        def resid_job(W, r0, nk, rhs):
            for n0 in range(0, D, 512):
                B = cx.bankset()
                wjob(cx, W, r0, nk, n0, 512, rhs, B)
                for j in range(4):
                    c = n0 // 128 + j
                    p.op("dve", lambda E, o=hT[:, c, :], a=B[j][:, :]: E.tensor_tensor(o, o, a, ALU.add),
                         reads=[B[j], hT.cells[c]], writes=[hT.cells[c]])

        xrhs = lambda kc: (x32[:, kc, :], [x32.cells[kc]])
        resid_job(w_o, 0, KC, xrhs)
        rmsnorm_T(cx, hT, g_ffn_t(gains, 0), x32, tmp)
        for half in range(2):
            hb = half * FH
            for n0 in range(0, FH, 512):
                w = min(512, FH - n0)
                G = cx.bankset(); wjob(cx, w_fg, 0, KC, hb + n0, w, xrhs, G)
                U = cx.bankset(); wjob(cx, w_fu, 0, KC, hb + n0, w, xrhs, U)
                for j in range(w // 128):
                    act_to(AF.Silu, sg[j], G[j])
                    c = n0 // 128 + j
                    p.op("dve", lambda E, o=big[:, c, :], a=U[j][:, :], s=sg[j][:]: E.tensor_tensor(o, a, s, ALU.mult),
                         reads=[U[j], sg[j]], writes=[big.cells[c]])
            resid_job(w_fd, hb, FHB, bigrhs(0))
        rmsnorm_T(cx, hT, g_ffn_t(gains, 1), x32, tmp)
        Gd = cx.bankset()
        wjob(cx, w_pgd, 0, KC, 0, 256, xrhs, Gd)
        for j in range(2):
            copy_op(p, "act", gdT[:, j, :], Gd[j][:, :], [Gd[j]], [gdT.cells[j]])
        for n0 in range(0, D, 512):
            G = cx.bankset(); wjob(cx, w_pgu, 0, 2, n0, 512, lambda kc: (gdT[:, kc, :], [gdT.cells[kc]]), G)
            P = cx.bankset(); wjob(cx, w_ple, 0, 2, n0, 512, lambda kc: (pT[:, kc, :], [pT.cells[kc]]), P)
            for j in range(4):
                act_to(AF.Sigmoid, sg[j], G[j])
                c = n0 // 128 + j
                x = nexttt()
                p.op("dve", lambda E, o=x[:], a=P[j][:, :], s=sg[j][:]: E.tensor_tensor(o, a, s, ALU.mult),
                     reads=[P[j], sg[j]], writes=[x])
                p.op("pool", lambda E, o=hT[:, c, :], s=x[:]: E.tensor_tensor(o, o, s, ALU.add),
                     reads=[hT.cells[c], x], writes=[hT.cells[c]])
        if final:
            rmsnorm_T(cx, hT, g_ffn_t(gains, 2), hT, tmp)
        for q in range(4):
            dst = outT_d[q * 1024:(q + 1) * 1024, cols].rearrange("(k p) t -> p k t", p=128)
            p.dma("sp", dst, hT[:, q * 8:(q + 1) * 8, :], hT, reads=hT.cells[q * 8:(q + 1) * 8])
    st = p.emit()
    return nc, st


class g_ffn_t:
    def __init__(self, gains, i):
        self.g = gains
        self.i = i
        self.cells = gains.cells

    def __getitem__(self, k):
        rows, colsl = k
        return self.g.t[rows, self.i * KC + colsl.start: self.i * KC + colsl.stop]

import math


def _t5_bucket_np(rel):
    n = np.maximum(rel, 0)
    nf = np.maximum(n, 1).astype(np.float32)
    large = 16 + (np.log(nf / np.float32(16)) / np.float32(math.log(8.0)) * np.float32(16)).astype(np.int32)
    large = np.minimum(large, 31)
    return np.where(n < 16, n, large)


def rep128(v):
    return np.ascontiguousarray(np.broadcast_to(np.asarray(v, np.float32).reshape(1, -1), (128, np.asarray(v).size)))


def b_consts(S):
    out = {}
    p_ = np.arange(128)[:, None, None]; j_ = np.arange(5)[None, :, None]; q_ = np.arange(512)[None, None, :]
    rel = q_ - p_ - 128 * (j_ - 1)
    out["rel"] = rel
    out["bucket"] = _t5_bucket_np(rel)
    pos = np.arange(S, dtype=np.float32)
    theta = (1.0 / (10000.0 ** np.linspace(0.0, 1.0, 64, dtype=np.float32))).astype(np.float32)
    ang = (pos[:, None] * theta[None, :]).astype(np.float32).astype(np.float64)
    c = np.cos(ang).T.astype(np.float32); s = np.sin(ang).T.astype(np.float32)
    out["cosF"] = np.ascontiguousarray(np.concatenate([c, c], 0))
    out["sinS"] = np.ascontiguousarray(np.concatenate([-s, s], 0))
    out["ident"] = np.eye(128, dtype=np.float32)
    g_ = np.arange(128) // 16
    out["bmask"] = np.ascontiguousarray((g_[:, None] == (np.arange(512) // 64)[None, :]).astype(np.float32))
    out["iota"] = rep128(np.arange(S5L + 1, dtype=np.float32))
    rc = []
    cv45 = []
    for h in range(8):
        lg = math.log1p(-2.0 ** (-5.0 - h))
        i_ = np.arange(128)
        d = i_[None, :] - i_[:, None]
        dm = np.where(d >= 0, np.exp(lg * np.maximum(d, 0)), 0.0) * SCALE
        xi = np.exp(lg * (i_ + 1.0))
        rc.append(np.concatenate([dm, np.broadcast_to(np.tile(xi, 4)[None, :], (128, 512))], 1).astype(np.float32))
        zeta = np.exp(lg * (127.0 - i_)) * SCALE
        cv45.append((zeta.astype(np.float32), np.float32(math.exp(lg * 128))))
    out["rconst"] = rc
    out["cv45"] = cv45
    return out


def b_inputs(layer, h, S, K, rel_bias, diff_lambda, diff_subln, s5p):
    lam_init = 0.8 - 0.6 * math.exp(-0.3 * layer)
    d = {}
    gathered = rel_bias[K["bucket"], h].astype(np.float32)
    d["btile"] = np.ascontiguousarray(np.where(K["rel"] >= 0, gathered, np.float32(-30000.0)).astype(np.float32))
    cv = np.zeros((128, 8), np.float32)
    cv[:, 0] = rel_bias[31, h]; cv[:, 1] = lam_init; cv[:, 2] = 1.0 - lam_init; cv[:, 3] = EPS
    cv[:, 4] = K["cv45"][h][0]; cv[:, 5] = K["cv45"][h][1]
    d["cvec"] = cv
    d["dlam"] = rep128(diff_lambda[layer].reshape(-1))
    d["subln"] = np.ascontiguousarray(diff_subln[layer].reshape(2, 128).T)
    d["cosF"] = K["cosF"]; d["sinS"] = K["sinS"]; d["rconst"] = K["rconst"][h]; d["ident"] = K["ident"]
    lre, lim, ldt, bre, bim, cre, cim, dsk = s5p
    g0 = 8 * h
    bj = np.zeros((128, 257), np.float32)
    for g in range(8):
        rows = slice(g * 16, (g + 1) * 16)
        bj[rows, 0:64] = lre[g0 + g][None, :]
        bj[rows, 64:128] = lim[g0 + g][None, :]
        bj[rows, 128:192] = bre[g0 + g].T
        bj[rows, 192:256] = bim[g0 + g].T
        bj[rows, 256] = ldt[g0 + g]
    d["s5bj"] = bj
    d["bmask"] = K["bmask"]
    st = np.zeros((128, 12), np.float32)
    cb = np.zeros((128, 8, 128), np.float32)
    for s_ in range(4):
        for gp in range(2):
            g = 2 * s_ + gp
            rows = slice(gp * 64, (gp + 1) * 64)
            st[rows, s_] = lre[g0 + g]; st[rows, 4 + s_] = lim[g0 + g]; st[rows, 8 + s_] = ldt[g0 + g]
            cb[rows, 2 * s_, g * 16:(g + 1) * 16] = cre[g0 + g].T
            cb[rows, 2 * s_ + 1, g * 16:(g + 1) * 16] = cim[g0 + g].T
    d["s5st"] = st; d["cblk"] = cb
    d["s5d"] = np.ascontiguousarray(dsk[128 * h:128 * (h + 1)].reshape(128, 1).astype(np.float32))
    d["iota"] = K["iota"]
    return d

from concourse.bass_utils import run_bass_kernel_spmd
import ml_dtypes

NCORES = 8
_PROGS = {}


def _prog(key, fn):
    if key not in _PROGS:
        _PROGS[key] = fn()[0]
    return _PROGS[key]


def _gl(g):
    return np.ascontiguousarray(np.asarray(g, np.float32).reshape(KC, 128).T)


def _c(a):
    return np.ascontiguousarray(a)


def kernel(x, p, rel_bias, norm_mix, w_in, diff_lambda, diff_subln,
           s5_lambda_re, s5_lambda_im, s5_log_dt, s5_b_re, s5_b_im, s5_c_re, s5_c_im,
           s5_d, s5_w_glu, w_gate_up, w_br_diff, w_br_ret, w_br_s5, w_o,
           norm_ffn, w_ffn_gate, w_ffn_up, w_ffn_down,
           norm_ple, w_ple, w_ple_gate_down, w_ple_gate_up, norm_final):
    f = lambda a: np.asarray(a, np.float32)
    x = f(x); p = f(p); rel_bias = f(rel_bias)
    S = x.shape[1]
    depth = int(np.asarray(w_in).shape[0])
    tcore = S // NCORES
    cores = list(range(NCORES))
    pa = _prog(("A", tcore), lambda: build_A(tcore))
    pb = _prog(("B", S), lambda: build_B(S))
    K = b_consts(S)
    hT = [_c(x[0, c * tcore:(c + 1) * tcore].T) for c in cores]
    for i in range(depth):
        wi = f(w_in[i]); g = _gl(norm_mix[i])
        ra = run_bass_kernel_spmd(pa, [{"hT": hT[c], "g": g, "w_in": wi} for c in cores], core_ids=cores).results
        zT = np.concatenate([r["zT"] for r in ra], axis=1)
        zv = np.concatenate([r["zv"] for r in ra], axis=0)
        s5p = tuple(f(a[i]) for a in (s5_lambda_re, s5_lambda_im, s5_log_dt, s5_b_re, s5_b_im, s5_c_re, s5_c_im, s5_d))
        ims = []
        for h in cores:
            d = b_inputs(i, h, S, K, rel_bias, f(diff_lambda), f(diff_subln), s5p)
            d["dqT"] = _c(zT[h * 256:(h + 1) * 256]); d["dkT"] = _c(zT[2048 + h * 256:2048 + (h + 1) * 256])
            d["dv"] = _c(zv[:, h * 256:(h + 1) * 256])
            d["rqT"] = _c(zT[4096 + h * 128:4096 + (h + 1) * 128]); d["rkT"] = _c(zT[5120 + h * 128:5120 + (h + 1) * 128])
            d["rv"] = _c(zv[:, 2048 + h * 128:2048 + (h + 1) * 128])
            d["rgT"] = _c(zT[6144 + h * 128:6144 + (h + 1) * 128]); d["suT"] = _c(zT[7168 + h * 128:7168 + (h + 1) * 128])
            ims.append(d)
        rb = run_bass_kernel_spmd(pb, ims, core_ids=cores).results
        odT = np.concatenate([r["odT"] for r in rb], axis=0)
        orT = np.concatenate([r["orT"] for r in rb], axis=0)
        ysT = np.concatenate([r["ysT"] for r in rb], axis=0)
        del rb
        final = (i == depth - 1)
        pc = _prog(("C", tcore, final), lambda: build_C(tcore, final))
        gains = _c(np.concatenate([_gl(norm_ffn[i]), _gl(norm_ple[i]), _gl(norm_final)], axis=1))
        W = dict(w_glu=f(s5_w_glu[i]), w_gu=f(w_gate_up[i]), w_bd=f(w_br_diff[i]), w_br=f(w_br_ret[i]), w_bs=f(w_br_s5[i]),
                 w_o=f(w_o[i]), w_fg=f(w_ffn_gate[i]), w_fu=f(w_ffn_up[i]), w_fd=f(w_ffn_down[i]),
                 w_ple=f(w_ple[i]), w_pgd=f(w_ple_gate_down[i]), w_pgu=f(w_ple_gate_up[i]))
        ims = []
        for c in cores:
            ts = slice(c * tcore, (c + 1) * tcore)
            d = dict(hT=hT[c], glT=_c(zT[8192:8448, ts]), odT=_c(odT[:, ts]), orT=_c(orT[:, ts]), ysT=_c(ysT[:, ts]),
                     pT=_c(p[i, 0, ts].T), gains=gains)
            d.update(W)
            ims.append(d)
        del zT, zv
        rc = run_bass_kernel_spmd(pc, ims, core_ids=cores).results
        hT = [r["outT"] for r in rc]
        del rc, ims
    out = np.concatenate([h_.T for h_ in hT], axis=0)[None]
    return np.ascontiguousarray(out.astype(np.float32))
```

```python
import numpy as np
import concourse.bass as bass
import concourse.mybir as mybir

F32 = mybir.dt.float32
BF16 = mybir.dt.bfloat16
ALU = mybir.AluOpType
AF = mybir.ActivationFunctionType
AX = mybir.AxisListType

ENGS = ("pe", "act", "dve", "pool", "sp")


class Cell:
    __slots__ = ("name", "lw", "rd", "T")

    def __init__(self, name, T):
        self.name = name
        self.lw = None
        self.rd = {}
        self.T = T


class Tens:
    def __init__(self, prog, name, handle, ncells):
        self.p = prog
        self.name = name
        self.t = handle
        self.cells = [Cell(f"{name}.{i}", self) for i in range(ncells)]
        self.dsem = None
        self.dcount = 0

    def __getitem__(self, k):
        return self.t[k]

    @property
    def c(self):
        return self.cells[0]


class Op:
    __slots__ = ("eng", "fn", "waits", "idx", "needed", "sig", "dma", "dsem", "dval", "T")

    def __init__(self, eng, fn, idx):
        self.eng = eng
        self.fn = fn
        self.idx = idx
        self.waits = []
        self.needed = False
        self.sig = None
        self.dma = False
        self.dsem = None
        self.dval = 0
        self.T = None


class Prog:
    def __init__(self, nc):
        self.nc = nc
        self.q = {e: [] for e in ENGS}
        self.sem = {e: nc.alloc_semaphore(f"sem_{e}") for e in ENGS}
        self.waited_c = {e: {x: -1 for x in ENGS} for e in ENGS}
        self.waited_d = {e: {} for e in ENGS}
        self.tens = []
        self.n_sb = 0
        self.stack = None
        self.signum = {e: 0 for e in ENGS}
        self.cleared = False
        self.nphase = 0

    def sb(self, name, shape, dtype, ncells=1):
        if self.stack is None:
            h = self.nc.alloc_sbuf_tensor(name, list(shape), dtype)
        else:
            h = self.stack.enter_context(self.nc.sbuf_tensor(f"{name}_ph{self.nphase}", list(shape), dtype))
        T = Tens(self, name, h, ncells)
        T.scoped = self.stack is not None
        self.tens.append(T)
        return T

    def phase_begin(self):
        import contextlib
        self.stack = contextlib.ExitStack()

    def phase_end(self):
        st = self.emit()
        self.stack.close()
        self.stack = None
        self.nphase += 1
        self.q = {e: [] for e in ENGS}
        self.waited_c = {e: {x: -1 for x in ENGS} for e in ENGS}
        self.tens = [T for T in self.tens if not getattr(T, "scoped", False)]
        for T in self.tens:
            for c in T.cells:
                c.lw = None
                c.rd = {}
        return st

    def ps(self, name, shape, dtype=F32, ncells=1):
        h = self.nc.alloc_psum_tensor(name, list(shape), dtype)
        T = Tens(self, name, h, ncells)
        self.tens.append(T)
        return T

    def _dsem(self, T):
        if T.dsem is None:
            T.dsem = self.nc.alloc_semaphore(f"dsem_{T.name}")
        return T.dsem

    def _need(self, op, prod):
        if prod is None:
            return
        e = op.eng
        if prod.dma:
            T = prod.T
            full = T.dcount
            cur = self.waited_d[e].get(id(T), 0)
            if cur >= prod.dval:
                return
            self.waited_d[e][id(T)] = full
            op.waits.append(("d", T, full))
        else:
            x = prod.eng
            if x == "pe" and e == "pe":
                return
            if self.waited_c[e][x] >= prod.idx:
                return
            self.waited_c[e][x] = prod.idx
            prod.needed = True
            op.waits.append(("c", prod))

    def _track(self, op, reads, writes):
        for c in reads:
            self._need(op, c.lw)
        for c in writes:
            self._need(op, c.lw)
            for r in c.rd.values():
                if r is not op:
                    self._need(op, r)
        key = op.eng if not op.dma else ("d", id(op.T))
        for c in reads:
            c.rd[key] = op
        for c in writes:
            c.lw = op
            c.rd = {}

    @staticmethod
    def _cells(xs):
        out = []
        for x in xs:
            if hasattr(x, "cells"):
                out.extend(x.cells)
            elif isinstance(x, Cell):
                out.append(x)
            else:
                out.extend(Prog._cells(x))
        return out

    def op(self, eng, fn, reads=(), writes=()):
        o = Op(eng, fn, len(self.q[eng]))
        self._track(o, self._cells(reads), self._cells(writes))
        self.q[eng].append(o)
        return o

    def dma(self, eng, out, in_, owner, reads=(), writes=(), **kw):
        o = Op(eng, None, len(self.q[eng]))
        o.dma = True
        o.T = owner
        o.dsem = self._dsem(owner)
        self._track(o, self._cells(reads), self._cells(writes))
        owner.dcount += 16
        o.dval = owner.dcount
        o.fn = lambda E, out=out, in_=in_, kw=kw: E.dma_start(out=out, in_=in_, **kw)
        self.q[eng].append(o)
        return o

    def emit(self, final_engine="sp"):
        nc = self.nc
        for e in ENGS:
            n = self.signum[e]
            for o in self.q[e]:
                if not o.dma and o.needed:
                    n += 1
                    o.sig = n
            self.signum[e] = n
        all_sems = [self.sem[e] for e in ENGS] + [T.dsem for T in self.tens if T.dsem is not None]
        dma_T = [T for T in self.tens if T.dsem is not None]
        stats = {e: len(self.q[e]) for e in ENGS}

        def replay(e, E):
            semE = self.sem[e]
            for o in self.q[e]:
                for w in o.waits:
                    if w[0] == "c":
                        E.wait_ge(self.sem[w[1].eng], w[1].sig)
                    else:
                        E.wait_ge(w[1].dsem, w[2])
                ins = o.fn(E)
                if o.dma:
                    ins.then_inc(o.dsem, 16)
                elif o.needed:
                    ins.then_inc(semE, 1)
            if e == final_engine:
                for T in dma_T:
                    E.wait_ge(T.dsem, T.dcount)

        if not self.cleared:
            self.cleared = True
            with nc.Block() as blk0:
                @blk0.sync
                def _(E):
                    for sm in [self.sem[e] for e in ENGS]:
                        E.sem_clear(sm)
        new_d = [T for T in dma_T if not getattr(T, "dclr", False)]
        if new_d:
            with nc.Block() as blk1:
                @blk1.sync
                def _(E):
                    for T in new_d:
                        E.sem_clear(T.dsem)
                        T.dclr = True

        with nc.Block() as blk:
            @blk.tensor
            def _(E):
                replay("pe", E)

            @blk.scalar
            def _(E):
                replay("act", E)

            @blk.vector
            def _(E):
                replay("dve", E)

            @blk.gpsimd
            def _(E):
                replay("pool", E)

            @blk.sync
            def _(E):
                replay("sp", E)
        return stats

D = 4096
KC = D // 128
TT = 512
EPS = 1e-6
IN_W = 11520
FFN = 11008
NSLOT = 6


class Ctx:
    def __init__(self, p, nslot=NSLOT):
        self.p = p
        self.nslot = nslot
        self.slots = [p.sb(f"wslot{i}", [128, 4, 512], BF16) for i in range(nslot)]
        self.si = 0
        self.banks = [p.ps(f"bank{i}", [128, 512], F32) for i in range(8)]
        self.ones = p.sb("ones_bf", [128, 128], BF16)
        p.op("dve", lambda E: E.memset(self.ones[:], 1.0), writes=[self.ones])
        self.flip = 0
        self.ev = 0

    def slot(self):
        s = self.slots[self.si % self.nslot]
        self.si += 1
        return s

    def bankset(self):
        b = self.banks[0:4] if self.flip == 0 else self.banks[4:8]
        self.flip ^= 1
        return b


def wjob(cx, W, r0, nkc, c0, ncols, rhs, banks, tm=False, ntok=TT):
    p = cx.p
    nb = (ncols + 127) // 128
    kc = 0
    while kc < nkc:
        g = min(4, nkc - kc)
        s = cx.slot()
        src = W[r0 + kc * 128: r0 + (kc + g) * 128, c0:c0 + ncols].rearrange("(k p) n -> p k n", p=128)
        p.dma("pool", s[:, 0:g, 0:ncols], src, s, writes=[s])
        for k in range(g):
            a_ap, a_cells = rhs(kc + k)
            first = (kc + k == 0)
            last = (kc + k == nkc - 1)
            if not tm:
                for j in range(nb):
                    w = min(128, ncols - j * 128)
                    p.op("pe", lambda E, o=banks[j][0:w, 0:ntok], l=s[:, k, j * 128:j * 128 + w], r=a_ap, f=first, la=last:
                         E.matmul(o, l, r, start=f, stop=la), reads=[s, a_cells], writes=[banks[j]])
            else:
                for j in range(ntok // 128):
                    p.op("pe", lambda E, o=banks[j][:, 0:ncols], l=a_ap[:, j * 128:(j + 1) * 128], r=s[:, k, 0:ncols], f=first, la=last:
                         E.matmul(o, l, r, start=f, stop=la), reads=[s, a_cells], writes=[banks[j]])
        kc += g


def evac_eng(cx):
    cx.ev ^= 1
    return "act" if cx.ev else "dve"


def copy_op(p, eng, out, in_, reads, writes):
    if eng == "act":
        p.op("act", lambda E: E.copy(out, in_), reads=reads, writes=writes)
    else:
        p.op(eng, lambda E: E.tensor_copy(out, in_), reads=reads, writes=writes)


def rmsnorm_T(cx, hT, gain, outT, tmp, ntok=TT, nkc=KC, dim=D):
    p = cx.p
    ss = cx.banks[7]
    for kc in range(nkc):
        sq = tmp["sq"][kc % 2]
        p.op("act", lambda E, o=sq[:, 0:ntok], i=hT[:, kc, 0:ntok]: E.activation(o, i, AF.Square),
             reads=[hT.cells[kc]], writes=[sq])
        p.op("pe", lambda E, r=sq[:, 0:ntok], f=(kc == 0), la=(kc == nkc - 1):
             E.matmul(ss[:, 0:ntok], cx.ones[:], r, start=f, stop=la), reads=[sq, cx.ones], writes=[ss])
    rs = tmp["rs"]
    p.op("act", lambda E: E.activation(rs[:, 0:ntok], ss[:, 0:ntok], AF.Sqrt, bias=tmp["eps"][:, 0:1], scale=1.0 / dim),
         reads=[ss, tmp["eps"]], writes=[rs])
    p.op("dve", lambda E: E.reciprocal(rs[:, 0:ntok], rs[:, 0:ntok]), reads=[rs], writes=[rs])
    for kc in range(nkc):
        eng = "dve"
        p.op(eng, lambda E, o=outT[:, kc, 0:ntok], i=hT[:, kc, 0:ntok], g=gain[:, kc:kc + 1]:
             E.scalar_tensor_tensor(o, i, g, rs[:, 0:ntok], ALU.mult, ALU.mult),
             reads=[hT.cells[kc], gain, rs], writes=[outT.cells[kc]])


def norm_tmp(p):
    t = {"sq": [p.sb(f"nsq{i}", [128, TT], BF16) for i in range(2)],
         "rs": p.sb("nrs", [128, TT], F32),
         "eps": p.sb("neps", [128, 1], F32)}
    p.op("dve", lambda E: E.memset(t["eps"][:], EPS), writes=[t["eps"]])
    return t

SEG_FM = [(0, 4096, 0), (6144, 2048, 4096), (9216, 2304, 6144)]
SEG_TM = [(4096, 2048, 0), (8192, 1024, 2048)]
ZT_ROWS = 8448
ZV_COLS = 3072


def build_A(tcore):
    nc = bass.Bass("TRN2", target_bir_lowering=False)
    hT_d = nc.dram_tensor("hT", [D, tcore], F32, kind="ExternalInput").ap()
    g_d = nc.dram_tensor("g", [128, KC], F32, kind="ExternalInput").ap()
    w_d = nc.dram_tensor("w_in", [D, IN_W], F32, kind="ExternalInput").ap()
    zT_d = nc.dram_tensor("zT", [ZT_ROWS, tcore], BF16, kind="ExternalOutput").ap()
    zv_d = nc.dram_tensor("zv", [tcore, ZV_COLS], BF16, kind="ExternalOutput").ap()
    p = Prog(nc)
    cx = Ctx(p)
    tmp = norm_tmp(p)
    hT = p.sb("hT_sb", [128, KC, TT], F32, ncells=KC)
    xn = p.sb("xn_sb", [128, KC, TT], BF16, ncells=KC)
    gain = p.sb("gain", [128, KC], F32)
    stg = [p.sb(f"stg{i}", [128, 4, 512], BF16) for i in range(2)]
    p.dma("sp", gain[:], g_d, gain, writes=[gain])
    si = 0
    for t in range(tcore // TT):
        t0 = t * TT
        for q in range(4):
            src = hT_d[q * 1024:(q + 1) * 1024, t0:t0 + TT].rearrange("(k p) t -> p k t", p=128)
            p.dma("sp", hT[:, q * 8:(q + 1) * 8, :], src, hT, writes=hT.cells[q * 8:(q + 1) * 8])
        rmsnorm_T(cx, hT, gain, xn, tmp)
        rhs = lambda kc: (xn[:, kc, :], [xn.cells[kc]])
        for (c0, ncols, off) in SEG_FM:
            for n0 in range(0, ncols, 512):
                w = min(512, ncols - n0)
                banks = cx.bankset()
                wjob(cx, w_d, 0, KC, c0 + n0, w, rhs, banks)
                s = stg[si % 2]; si += 1
                nb = w // 128
                for j in range(nb):
                    copy_op(p, evac_eng(cx), s[:, j, :], banks[j][:, :], [banks[j]], [s])
                dst = zT_d[off + n0: off + n0 + w, t0:t0 + TT].rearrange("(j p) t -> p j t", p=128)
                p.dma("sp", dst, s[:, 0:nb, :], s, reads=[s])
        for (c0, ncols, off) in SEG_TM:
            for n0 in range(0, ncols, 512):
                banks = cx.bankset()
                wjob(cx, w_d, 0, KC, c0 + n0, 512, rhs, banks, tm=True)
                s = stg[si % 2]; si += 1
                for j in range(4):
                    copy_op(p, evac_eng(cx), s[:, j, :], banks[j][:, :], [banks[j]], [s])
                dst = zv_d[t0:t0 + TT, off + n0: off + n0 + 512].rearrange("(j p) n -> p j n", p=128)
                p.dma("sp", dst, s[:, :, :], s, reads=[s])
    st = p.emit()
    return nc, st

QB = 512
SCALE = 128 ** -0.5
S5L = 256


def build_B(S, parts=("diff", "ret", "s5")):
    nc = bass.Bass("TRN2", target_bir_lowering=False)
    di = lambda n, s, dt=F32: nc.dram_tensor(n, s, dt, kind="ExternalInput").ap()
    do = lambda n, s, dt=BF16: nc.dram_tensor(n, s, dt, kind="ExternalOutput").ap()
    NKB = S // 128
    dqT = di("dqT", [256, S], BF16); dkT = di("dkT", [256, S], BF16); dv = di("dv", [S, 256], BF16)
    btile = di("btile", [128, 5, 512]); cvec = di("cvec", [128, 8]); dlam = di("dlam", [128, 512]); subln = di("subln", [128, 2])
    rqT = di("rqT", [128, S], BF16); rkT = di("rkT", [128, S], BF16); rv = di("rv", [S, 128], BF16); rgT = di("rgT", [128, S], BF16)
    cosF = di("cosF", [128, S]); sinS = di("sinS", [128, S]); rconst = di("rconst", [128, 128 + 512]); ident_d = di("ident", [128, 128])
    suT = di("suT", [128, S], BF16)
    s5bj = di("s5bj", [128, 64 * 4 + 1]); bmask = di("bmask", [128, 512]); s5st = di("s5st", [128, 12]); cblk = di("cblk", [128, 8, 128])
    s5d = di("s5d", [128, 1]); iota = di("iota", [128, S5L + 1])
    odT = do("odT", [256, S]); orT = do("orT", [128, S]); ysT = do("ysT", [128, S])

    p = Prog(nc)
    cx = Ctx(p, nslot=0)
    bk = cx.banks
    cv = p.sb("cvec_sb", [128, 8], F32)
    p.dma("sp", cv[:], cvec, cv, writes=[cv])
    ident = p.sb("ident_f", [128, 128], F32); identb = p.sb("ident_b", [128, 128], BF16)
    p.dma("sp", ident[:], ident_d, ident, writes=[ident])
    p.op("dve", lambda E: E.tensor_copy(identb[:], ident[:]), reads=[ident], writes=[identb])

    def vop(eng, fn, reads, writes):
        p.op(eng, fn, reads=reads, writes=writes)

    def tt_op(eng, o, a, b, op, reads, writes):
        p.op(eng, lambda E: E.tensor_tensor(o, a, b, op), reads=reads, writes=writes)

    if "diff" in parts:
        p.phase_begin()
        kT = [p.sb(f"kT{m}", [128, S], BF16) for m in range(2)]
        vv = p.sb("vv", [128, NKB, 256], BF16)
        bt = p.sb("bt", [128, 5, 512], F32)
        lamt = p.sb("lamt", [128, 512], F32); lw = p.sb("lw", [128, 256], F32); ls = p.sb("ls", [128, 4], F32)
        sl = p.sb("subln_sb", [128, 2], F32)
        qt = [[p.sb(f"qt{b}{m}", [128, QB], BF16) for m in range(2)] for b in range(2)]
        pt = [p.sb(f"pt{i}", [128, QB], BF16) for i in range(3)]
        nt = [p.sb(f"nt{i}", [128, QB], F32) for i in range(2)]
        rsd = p.sb("rsd", [128, QB], F32)
        ost = [p.sb(f"ost{i}", [128, 2, QB], BF16) for i in range(2)]
        for m in range(2):
            for hh in range(0, S, 4096):
                w = min(4096, S - hh)
                p.dma("sp", kT[m][:, hh:hh + w], dkT[m * 128:(m + 1) * 128, hh:hh + w], kT[m], writes=[kT[m]])
        for hh in range(0, NKB, 32):
            w = min(32, NKB - hh)
            p.dma("sp", vv[:, hh:hh + w, :], dv[hh * 128:(hh + w) * 128, :].rearrange("(b p) e -> p b e", p=128), vv, writes=[vv])
        p.dma("sp", bt[:], btile, bt, writes=[bt])
        p.dma("sp", lamt[:], dlam, lamt, writes=[lamt])
        p.dma("sp", sl[:], subln, sl, writes=[sl])
        tt_op("dve", lw[:, 0:128], lamt[:, 0:128], lamt[:, 128:256], ALU.mult, [lamt], [lw])
        tt_op("dve", lw[:, 128:256], lamt[:, 256:384], lamt[:, 384:512], ALU.mult, [lamt], [lw])
        vop("dve", lambda E: E.reduce_sum(ls[:, 0:1], lw[:, 0:128], AX.X), [lw], [ls])
        vop("dve", lambda E: E.reduce_sum(ls[:, 1:2], lw[:, 128:256], AX.X), [lw], [ls])
        vop("act", lambda E: E.activation(ls[:, 0:2], ls[:, 0:2], AF.Exp), [ls], [ls])
        tt_op("dve", ls[:, 2:3], ls[:, 1:2], ls[:, 0:1], ALU.subtract, [ls], [ls])
        tt_op("dve", ls[:, 2:3], ls[:, 2:3], cv[:, 1:2], ALU.subtract, [ls, cv], [ls])
        vop("dve", lambda E: E.tensor_scalar(sl[:], sl[:], cv[:, 2:3], None, ALU.mult), [sl, cv], [sl])
        acp = [[p.sb(f"acp{b_}{i}", [128, QB], F32) for i in range(6)] for b_ in range(2)]
        oa2 = [[p.sb(f"oa2{b_}{i}", [128, QB], F32) for i in range(2)] for b_ in range(2)]
        sq2 = [[p.sb(f"sq2{b_}{i}", [128, QB], BF16) for i in range(2)] for b_ in range(2)]
        cnt = {"sc": 0, "p": 0}
        NQB = S // QB
        items = []
        for qb in range(NQB):
            nkb = qb * 4 + 4
            for m in range(2):
                for kb in range(nkb):
                    items.append((qb, m, kb, nkb))
        qtiles = {}

        def load_q(qb):
            qq = qt[qb % 2]
            for m in range(2):
                p.dma("sp", qq[m][:], dqT[m * 128:(m + 1) * 128, qb * QB:(qb + 1) * QB], qq[m], writes=[qq[m]])
            qtiles[qb] = qq

        def score_stage(it):
            qb, m, kb, nkb = it
            if qb not in qtiles:
                load_q(qb)
            qq = qtiles[qb]
            q0 = qb * QB
            j = kb - q0 // 128
            qlo = max(0, 128 * j)
            sc = bk[6 + (cnt["sc"] % 2)]; cnt["sc"] += 1
            vop("pe", lambda E, o=sc[:, qlo:QB], l=kT[m][:, kb * 128:(kb + 1) * 128], rr=qq[m][:, qlo:QB]:
                E.matmul(o, l, rr, start=True, stop=True), [kT[m], qq[m]], [sc])
            P = pt[cnt["p"] % 3]; cnt["p"] += 1
            if j < -1:
                vop("act", lambda E, o=P[:, qlo:QB], i=sc[:, qlo:QB]:
                    E.activation(o, i, AF.Exp, bias=cv[:, 0:1], scale=SCALE), [sc, cv], [P])
            else:
                N = nt[kb % 2]
                vop("dve", lambda E, o=N[:, qlo:QB], i=sc[:, qlo:QB], b=bt[:, j + 1, qlo:QB]:
                    E.scalar_tensor_tensor(o, i, SCALE, b, ALU.mult, ALU.add), [sc, bt], [N])
                vop("act", lambda E, o=P[:, qlo:QB], i=N[:, qlo:QB]: E.activation(o, i, AF.Exp), [N], [P])
            return P, qlo

        def pv_stage(it, P, qlo):
            qb, m, kb, nkb = it
            accs = (bk[3 * m], bk[3 * m + 1], bk[3 * m + 2])
            f, la = (kb == 0), (kb == nkb - 1)
            for es in range(2):
                vop("pe", lambda E, o=accs[es][:, qlo:QB], l=vv[:, kb, es * 128:(es + 1) * 128], rr=P[:, qlo:QB], f=f, la=la:
                    E.matmul(o, l, rr, start=f, stop=la), [vv, P], [accs[es]])
            vop("pe", lambda E, o=accs[2][:, qlo:QB], rr=P[:, qlo:QB], f=f, la=la:
                E.matmul(o, cx.ones[:], rr, start=f, stop=la), [cx.ones, P], [accs[2]])

        def epi1(qb):
            A_ = acp[qb % 2]; OA = oa2[qb % 2]; SQ = sq2[qb % 2]
            for i in range(6):
                vop("dve", lambda E, o=A_[i][:], s_=bk[i][:, :]: E.tensor_copy(o, s_), [bk[i]], [A_[i]])
            vop("dve", lambda E, x_=A_[2]: E.reciprocal(x_[:], x_[:]), [A_[2]], [A_[2]])
            vop("dve", lambda E, x_=A_[5]: E.reciprocal(x_[:], x_[:]), [A_[5]], [A_[5]])
            vop("dve", lambda E, x_=A_[5]: E.tensor_scalar(x_[:], x_[:], ls[:, 2:3], None, ALU.mult), [A_[5], ls], [A_[5]])
            for es in range(2):
                tt_op("pool", OA[es][:], A_[es][:], A_[2][:], ALU.mult, [A_[es], A_[2]], [OA[es]])
                tt_op("pool", A_[3 + es][:], A_[3 + es][:], A_[5][:], ALU.mult, [A_[3 + es], A_[5]], [A_[3 + es]])
                tt_op("pool", OA[es][:], OA[es][:], A_[3 + es][:], ALU.add, [OA[es], A_[3 + es]], [OA[es]])
                tt_op("pool", SQ[es][:], OA[es][:], OA[es][:], ALU.mult, [OA[es]], [SQ[es]])

        def epi2(qb):
            OA = oa2[qb % 2]; SQ = sq2[qb % 2]
            sb_ = bk[6 + (cnt["sc"] % 2)]; cnt["sc"] += 1
            for es in range(2):
                vop("pe", lambda E, o=sb_[:, :], rr=SQ[es][:], f=(es == 0), la=(es == 1):
                    E.matmul(o, cx.ones[:], rr, start=f, stop=la), [cx.ones, SQ[es]], [sb_])
            vop("act", lambda E, i=sb_[:, :]: E.activation(rsd[:], i, AF.Sqrt, bias=cv[:, 3:4], scale=1.0 / 256), [sb_, cv], [rsd])
            vop("dve", lambda E: E.reciprocal(rsd[:], rsd[:]), [rsd], [rsd])
            O = ost[qb % 2]
            for es in range(2):
                vop("dve", lambda E, o=O[:, es, :], i=OA[es][:], g=sl[:, es:es + 1]:
                    E.scalar_tensor_tensor(o, i, g, rsd[:], ALU.mult, ALU.mult), [OA[es], sl, rsd], [O])
            p.dma("sp", odT[:, qb * QB:(qb + 1) * QB].rearrange("(e p) t -> p e t", p=128), O[:, :, :], O, reads=[O])

        pend = None
        cur = score_stage(items[0])
        for i, it in enumerate(items):
            nxt = score_stage(items[i + 1]) if i + 1 < len(items) else None
            pv_stage(it, *cur)
            cur = nxt
            qb, m, kb, nkb = it
            if pend is not None:
                pend[1] -= 1
                if pend[1] <= 0:
                    epi2(pend[0]); pend = None
            if m == 1 and kb == nkb - 1:
                if pend is not None:
                    epi2(pend[0]); pend = None
                epi1(qb)
                pend = [qb, 6]
        if pend is not None:
            epi2(pend[0])
        p.phase_end()

    if "ret" in parts:
        p.phase_begin()
        rc = p.sb("rconst_sb", [128, 128 + 512], F32)
        p.dma("sp", rc[:], rconst, rc, writes=[rc])
        NB = 2
        rq = [p.sb(f"rq{i}", [128, 512], BF16) for i in range(NB)]; rqs = [p.sb(f"rqs{i}", [128, 512], BF16) for i in range(NB)]
        rk = [p.sb(f"rk{i}", [128, 512], BF16) for i in range(NB)]; rks = [p.sb(f"rks{i}", [128, 512], BF16) for i in range(NB)]
        rg = [p.sb(f"rg{i}", [128, 512], BF16) for i in range(NB)]
        rvv = [p.sb(f"rvv{i}", [128, 4, 128], BF16) for i in range(NB)]
        cs = [p.sb(f"cs{i}", [128, 512], F32) for i in range(NB)]; sn = [p.sb(f"sn{i}", [128, 512], F32) for i in range(NB)]
        ta = p.sb("r_ta", [128, 512], F32); tb = p.sb("r_tb", [128, 512], F32)
        qr = p.sb("r_qr", [128, 512], BF16); kr = p.sb("r_kr", [128, 512], BF16); qx = p.sb("r_qx", [128, 512], BF16)
        kz = [p.sb(f"r_kz{i}", [128, 128], BF16) for i in range(2)]
        inT = [p.sb(f"r_inT{i}", [128, 128], BF16) for i in range(2)]
        R = p.sb("r_R", [128, 128], F32); Rb = [p.sb(f"r_Rb{i}", [128, 128], BF16) for i in range(2)]
        sqr = p.sb("r_sq", [128, 512], BF16); rsr = p.sb("r_rs", [128, 512], F32); sgr = p.sb("r_sg", [128, 512], F32)
        orr = p.sb("r_or", [128, 512], F32); oro = [p.sb(f"r_oo{i}", [128, 512], BF16) for i in range(2)]
        vop("dve", lambda E: E.memset(R[:], 0.0), [], [R])
        nch = 0
        for sc_i in range(S // 512):
            t0 = sc_i * 512
            b = sc_i % NB
            cols = slice(t0, t0 + 512)
            p.dma("sp", rq[b][:], rqT[:, cols], rq[b], writes=[rq[b]])
            p.dma("sp", rqs[b][0:64, :], rqT[64:128, cols], rqs[b], writes=[rqs[b]])
            p.dma("sp", rqs[b][64:128, :], rqT[0:64, cols], rqs[b], writes=[rqs[b]])
            p.dma("sp", rk[b][:], rkT[:, cols], rk[b], writes=[rk[b]])
            p.dma("sp", rks[b][0:64, :], rkT[64:128, cols], rks[b], writes=[rks[b]])
            p.dma("sp", rks[b][64:128, :], rkT[0:64, cols], rks[b], writes=[rks[b]])
            p.dma("sp", rg[b][:], rgT[:, cols], rg[b], writes=[rg[b]])
            p.dma("sp", rvv[b][:], rv[cols, :].rearrange("(c p) e -> p c e", p=128), rvv[b], writes=[rvv[b]])
            p.dma("sp", cs[b][:], cosF[:, cols], cs[b], writes=[cs[b]])
            p.dma("sp", sn[b][:], sinS[:, cols], sn[b], writes=[sn[b]])
            tt_op("dve", ta[:], rq[b][:], cs[b][:], ALU.mult, [rq[b], cs[b]], [ta])
            tt_op("pool", tb[:], rqs[b][:], sn[b][:], ALU.mult, [rqs[b], sn[b]], [tb])
            tt_op("dve", qr[:], ta[:], tb[:], ALU.add, [ta, tb], [qr])
            tt_op("pool", qx[:], qr[:], rc[:, 128:640], ALU.mult, [qr, rc], [qx])
            tt_op("dve", ta[:], rk[b][:], cs[b][:], ALU.mult, [rk[b], cs[b]], [ta])
            tt_op("pool", tb[:], rks[b][:], sn[b][:], ALU.mult, [rks[b], sn[b]], [tb])
            tt_op("dve", kr[:], ta[:], tb[:], ALU.add, [ta, tb], [kr])
            ops = bk[sc_i % 2]
            for ch in range(4):
                cc = slice(ch * 128, (ch + 1) * 128)
                ib = bk[2 + (nch % 2)]
                vop("pe", lambda E, o=ib[:, 0:128], l=kr[:, cc], rr=qr[:, cc]: E.matmul(o, l, rr, start=True, stop=True), [kr, qr], [ib])
                I = inT[nch % 2]
                tt_op("dve", I[:], ib[:, 0:128], rc[:, 0:128], ALU.mult, [ib, rc], [I])
                first = (nch == 0)
                vop("pe", lambda E, o=ops[:, cc], l=rvv[b][:, ch, :], rr=I[:], la=first: E.matmul(o, l, rr, start=True, stop=la),
                    [rvv[b], I], [ops])
                if not first:
                    vop("pe", lambda E, o=ops[:, cc], l=Rb[nch % 2][:], rr=qx[:, cc]: E.matmul(o, l, rr, start=False, stop=True),
                        [Rb[nch % 2], qx], [ops])
                vop("pe", lambda E, l=kr[:, cc]: E.matmul(bk[5][:, 0:128], l, identb[:], start=True, stop=True), [kr, identb], [bk[5]])
                KZ = kz[nch % 2]
                vop("dve", lambda E, o=KZ[:]: E.tensor_scalar(o, bk[5][:, 0:128], cv[:, 4:5], None, ALU.mult), [bk[5], cv], [KZ])
                vop("pe", lambda E, l=KZ[:], rr=rvv[b][:, ch, :]: E.matmul(bk[4][:, 0:128], l, rr, start=True, stop=True), [KZ, rvv[b]], [bk[4]])
                vop("dve", lambda E: E.scalar_tensor_tensor(R[:], R[:], cv[:, 5:6], bk[4][:, 0:128], ALU.mult, ALU.add), [R, cv, bk[4]], [R])
                nch += 1
                vop("pool", lambda E, o=Rb[nch % 2][:]: E.tensor_copy(o, R[:]), [R], [Rb[nch % 2]])
            vop("act", lambda E, ops=ops: E.activation(sqr[:], ops[:, :], AF.Square), [ops], [sqr])
            vop("pe", lambda E: E.matmul(bk[6][:, :], cx.ones[:], sqr[:], start=True, stop=True), [cx.ones, sqr], [bk[6]])
            vop("act", lambda E: E.activation(rsr[:], bk[6][:, :], AF.Sqrt, bias=cv[:, 3:4], scale=1.0 / 128), [bk[6], cv], [rsr])
            vop("dve", lambda E: E.reciprocal(rsr[:], rsr[:]), [rsr], [rsr])
            vop("act", lambda E, g_=rg[b]: E.activation(sgr[:], g_[:], AF.Silu), [rg[b]], [sgr])
            tt_op("dve", orr[:], ops[:, :], rsr[:], ALU.mult, [ops, rsr], [orr])
            OO = oro[sc_i % 2]
            tt_op("pool", OO[:], orr[:], sgr[:], ALU.mult, [orr, sgr], [OO])
            p.dma("sp", orT[:, cols], OO[:], OO, reads=[OO])
        p.phase_end()

    if "s5" in parts:
        p.phase_begin()
        L = S5L
        bj = p.sb("s5bj_sb", [128, 257], F32)
        st = p.sb("s5st_sb", [128, 12], F32)
        bm = p.sb("bmask_sb", [128, 512], F32)
        cb = p.sb("cblk_f", [128, 8, 128], F32); cbb = p.sb("cblk_b", [128, 8, 128], BF16)
        dsk = p.sb("s5d_sb", [128, 1], F32); io = p.sb("iota_sb", [128, L + 1], F32)
        for (T_, src) in ((bj, s5bj), (st, s5st), (bm, bmask), (cb, cblk), (dsk, s5d), (io, iota)):
            p.dma("sp", T_[:], src, T_, writes=[T_])
        vop("pool", lambda E: E.tensor_copy(cbb[:], cb[:]), [cb], [cbb])
        TWO_PI = 2.0 * np.pi

        MAGIC = 12582912.0

        def frac(dst, src, scr, shape):
            vop("dve", lambda E: E.tensor_scalar(scr[:], src[:], MAGIC, None, ALU.add), [src], [scr])
            vop("dve", lambda E: E.tensor_scalar(scr[:], scr[:], -MAGIC, None, ALU.add), [scr], [scr])
            tt_op("dve", dst[:], src[:], scr[:], ALU.subtract, [src, scr], [dst])

        def disc(n, lr, li, ldt, pre, src, scalar_dt=False):
            o = {k: p.sb(f"{pre}_{k}", [128, n], F32) for k in ("dt", "mag", "th", "ar", "ai", "t1", "t2")}
            ndt = 1 if scalar_dt else n
            dt_ = p.sb(f"{pre}_dtt", [128, ndt], F32)
            vop("act", lambda E: E.activation(dt_[:], ldt, AF.Exp), [src], [dt_])
            if scalar_dt:
                vop("dve", lambda E: E.tensor_scalar(o["mag"][:], lr, dt_[:, 0:1], None, ALU.mult), [dt_, src], [o["mag"]])
            else:
                tt_op("dve", o["mag"][:], lr, dt_[:], ALU.mult, [dt_, src], [o["mag"]])
            vop("act", lambda E: E.activation(o["mag"][:], o["mag"][:], AF.Exp), [o["mag"]], [o["mag"]])
            if scalar_dt:
                vop("dve", lambda E: E.tensor_scalar(o["th"][:], li, dt_[:, 0:1], None, ALU.mult), [dt_, src], [o["th"]])
            else:
                tt_op("dve", o["th"][:], li, dt_[:], ALU.mult, [dt_, src], [o["th"]])
            vop("dve", lambda E: E.tensor_scalar(o["th"][:], o["th"][:], 1.0 / TWO_PI, None, ALU.mult), [o["th"]], [o["th"]])
            frac(o["th"], o["th"], o["t1"], [128, n])
            vop("act", lambda E: E.activation(o["t1"][:], o["th"][:], AF.Sin, scale=TWO_PI), [o["th"]], [o["t1"]])
            tt_op("dve", o["ai"][:], o["t1"][:], o["mag"][:], ALU.mult, [o["t1"], o["mag"]], [o["ai"]])
            vop("dve", lambda E: E.tensor_scalar(o["t2"][:], o["th"][:], 0.25, None, ALU.add), [o["th"]], [o["t2"]])
            frac(o["t2"], o["t2"], o["t1"], [128, n])
            vop("act", lambda E: E.activation(o["t2"][:], o["t2"][:], AF.Sin, scale=TWO_PI), [o["t2"]], [o["t2"]])
            tt_op("dve", o["ar"][:], o["t2"][:], o["mag"][:], ALU.mult, [o["t2"], o["mag"]], [o["ar"]])
            return o
        dj = disc(64, bj[:, 0:64], bj[:, 64:128], bj[:, 256:257], "dj", bj, scalar_dt=True)
        den = p.sb("dj_den", [128, 64], F32); fr = p.sb("dj_fr", [128, 64], F32); fi = p.sb("dj_fi", [128, 64], F32)
        am1 = p.sb("dj_am1", [128, 64], F32); tq = p.sb("dj_tq", [128, 64], F32)
        lr, li = bj[:, 0:64], bj[:, 64:128]
        tt_op("dve", den[:], lr, lr, ALU.mult, [bj], [den])
        tt_op("dve", tq[:], li, li, ALU.mult, [bj], [tq])
        tt_op("dve", den[:], den[:], tq[:], ALU.add, [den, tq], [den])
        vop("dve", lambda E: E.reciprocal(den[:], den[:]), [den], [den])
        vop("dve", lambda E: E.tensor_scalar(am1[:], dj["ar"][:], -1.0, None, ALU.add), [dj["ar"]], [am1])
        tt_op("dve", fr[:], am1[:], lr, ALU.mult, [am1, bj], [fr])
        tt_op("dve", tq[:], dj["ai"][:], li, ALU.mult, [dj["ai"], bj], [tq])
        tt_op("dve", fr[:], fr[:], tq[:], ALU.add, [fr, tq], [fr])
        tt_op("dve", fr[:], fr[:], den[:], ALU.mult, [fr, den], [fr])
        tt_op("dve", fi[:], dj["ai"][:], lr, ALU.mult, [dj["ai"], bj], [fi])
        tt_op("dve", tq[:], am1[:], li, ALU.mult, [am1, bj], [tq])
        tt_op("dve", fi[:], fi[:], tq[:], ALU.subtract, [fi, tq], [fi])
        tt_op("dve", fi[:], fi[:], den[:], ALU.mult, [fi, den], [fi])
        bbr = p.sb("bbr", [128, 64], F32); bbi = p.sb("bbi", [128, 64], F32)
        bre, bim = bj[:, 128:192], bj[:, 192:256]
        tt_op("dve", bbr[:], fr[:], bre, ALU.mult, [fr, bj], [bbr])
        tt_op("dve", tq[:], fi[:], bim, ALU.mult, [fi, bj], [tq])
        tt_op("dve", bbr[:], bbr[:], tq[:], ALU.subtract, [bbr, tq], [bbr])
        tt_op("dve", bbi[:], fr[:], bim, ALU.mult, [fr, bj], [bbi])
        tt_op("dve", tq[:], fi[:], bre, ALU.mult, [fi, bj], [tq])
        tt_op("dve", bbi[:], bbi[:], tq[:], ALU.add, [bbi, tq], [bbi])
        Bre = p.sb("Bre", [128, 512], BF16); Bim = p.sb("Bim", [128, 512], BF16)
        for g_ in range(8):
            gs = slice(g_ * 64, (g_ + 1) * 64)
            tt_op("dve", Bre[:, gs], bm[:, gs], bbr[:], ALU.mult, [bm, bbr], [Bre])
            tt_op("dve", Bim[:, gs], bm[:, gs], bbi[:], ALU.mult, [bm, bbi], [Bim])
        ds = disc(4, st[:, 0:4], st[:, 4:8], st[:, 8:12], "ds", st)
        cosT = p.sb("cosT", [128, 4, L + 1], F32); sinT = p.sb("sinT", [128, 4, L + 1], F32); mt = p.sb("mt", [128, 4, L], F32)
        ang = p.sb("ang", [128, L + 1], F32); ang2 = p.sb("ang2", [128, L + 1], F32); ang3 = p.sb("ang3", [128, L + 1], F32)
        for s_ in range(4):
            th = ds["th"][:, s_:s_ + 1]
            vop("dve", lambda E, th=th: E.tensor_scalar(ang[:], io[:], th, None, ALU.mult), [io, ds["th"]], [ang])
            frac(ang2, ang, ang3, None)
            vop("act", lambda E, s_=s_: E.activation(sinT[:, s_, :], ang2[:], AF.Sin, scale=TWO_PI), [ang2], [sinT])
            vop("dve", lambda E: E.tensor_scalar(ang[:], ang[:], 0.25, None, ALU.add), [ang], [ang])
            frac(ang2, ang, ang3, None)
            vop("act", lambda E, s_=s_: E.activation(cosT[:, s_, :], ang2[:], AF.Sin, scale=TWO_PI), [ang2], [cosT])
            vop("dve", lambda E, s_=s_: E.memset(mt[:, s_, :], 1.0), [], [mt])
            vop("dve", lambda E, s_=s_: E.tensor_scalar(mt[:, s_, :], mt[:, s_, :], ds["mag"][:, s_:s_ + 1], None, ALU.mult), [mt, ds["mag"]], [mt])
        ub = [p.sb(f"s5u{i}", [128, L], BF16) for i in range(2)]
        wt_ = {k: p.sb(f"s5_{k}", [128, L], F32) for k in ("t1", "t2", "t3", "t4", "wr", "wi", "gr", "gi", "u1", "u2", "u3", "u4", "y", "x2", "sg")}
        hrb = [p.sb(f"s5hr{i}", [128, L], BF16) for i in range(2)]; hib = [p.sb(f"s5hi{i}", [128, L], BF16) for i in range(2)]
        ini = p.sb("s5ini", [128, 4, 2], F32); tini = p.sb("s5tini", [128, 1], F32)
        yo = [p.sb(f"s5yo{i}", [128, L], BF16) for i in range(2)]
        vop("dve", lambda E: E.memset(ini[:], 0.0), [], [ini])
        W = wt_
        for c_ in range(S // L):
            cols = slice(c_ * L, (c_ + 1) * L)
            U = ub[c_ % 2]
            p.dma("sp", U[:], suT[:, cols], U, writes=[U])
            yps = bk[4 + (c_ % 2)]
            for s_ in range(4):
                xb = bk[s_]
                vop("pe", lambda E, s_=s_, xb=xb, U=U: E.matmul(xb[:, 0:L], Bre[:, s_ * 128:(s_ + 1) * 128], U[:], start=True, stop=True), [Bre, U], [xb])
                vop("pe", lambda E, s_=s_, xb=xb, U=U: E.matmul(xb[:, L:2 * L], Bim[:, s_ * 128:(s_ + 1) * 128], U[:], start=True, stop=True), [Bim, U], [xb])
                co, si = cosT[:, s_, 0:L], sinT[:, s_, 0:L]
                xr, xi_ = xb[:, 0:L], xb[:, L:2 * L]
                tt_op("dve", W["t1"][:], xr, co, ALU.mult, [xb, cosT], [W["t1"]])
                tt_op("dve", W["t2"][:], xi_, si, ALU.mult, [xb, sinT], [W["t2"]])
                tt_op("pool", W["wr"][:], W["t1"][:], W["t2"][:], ALU.add, [W["t1"], W["t2"]], [W["wr"]])
                tt_op("dve", W["t3"][:], xi_, co, ALU.mult, [xb, cosT], [W["t3"]])
                tt_op("dve", W["t4"][:], xr, si, ALU.mult, [xb, sinT], [W["t4"]])
                tt_op("pool", W["wi"][:], W["t3"][:], W["t4"][:], ALU.subtract, [W["t3"], W["t4"]], [W["wi"]])
                vop("dve", lambda E, s_=s_: E.tensor_tensor_scan(W["gr"][:], mt[:, s_, :], W["wr"][:], ini[:, s_, 0:1], ALU.mult, ALU.add),
                    [mt, W["wr"], ini], [W["gr"]])
                vop("dve", lambda E, s_=s_: E.tensor_tensor_scan(W["gi"][:], mt[:, s_, :], W["wi"][:], ini[:, s_, 1:2], ALU.mult, ALU.add),
                    [mt, W["wi"], ini], [W["gi"]])
                cL, sL = cosT[:, s_, L:L + 1], sinT[:, s_, L:L + 1]
                grl, gil = W["gr"][:, L - 1:L], W["gi"][:, L - 1:L]
                vop("dve", lambda E, gil=gil, sL=sL: E.tensor_scalar(tini[:], gil, sL, None, ALU.mult), [W["gi"], sinT], [tini])
                vop("dve", lambda E, s_=s_, grl=grl, cL=cL: E.scalar_tensor_tensor(ini[:, s_, 0:1], grl, cL, tini[:], ALU.mult, ALU.subtract),
                    [W["gr"], cosT, tini], [ini])
                vop("dve", lambda E, grl=grl, sL=sL: E.tensor_scalar(tini[:], grl, sL, None, ALU.mult), [W["gr"], sinT], [tini])
                vop("dve", lambda E, s_=s_, gil=gil, cL=cL: E.scalar_tensor_tensor(ini[:, s_, 1:2], gil, cL, tini[:], ALU.mult, ALU.add),
                    [W["gi"], cosT, tini], [ini])
                HR, HI = hrb[s_ % 2], hib[s_ % 2]
                tt_op("pool", W["u1"][:], W["gr"][:], co, ALU.mult, [W["gr"], cosT], [W["u1"]])
                tt_op("pool", W["u2"][:], W["gi"][:], si, ALU.mult, [W["gi"], sinT], [W["u2"]])
                tt_op("pool", HR[:], W["u1"][:], W["u2"][:], ALU.subtract, [W["u1"], W["u2"]], [HR])
                tt_op("pool", W["u3"][:], W["gr"][:], si, ALU.mult, [W["gr"], sinT], [W["u3"]])
                tt_op("pool", W["u4"][:], W["gi"][:], co, ALU.mult, [W["gi"], cosT], [W["u4"]])
                vop("dve", lambda E, HI=HI: E.scalar_tensor_tensor(HI[:], W["u3"][:], -1.0, W["u4"][:], ALU.mult, ALU.subtract),
                    [W["u3"], W["u4"]], [HI])
                vop("pe", lambda E, s_=s_, HR=HR, yps=yps: E.matmul(yps[:, 0:L], cbb[:, 2 * s_, :], HR[:], start=(s_ == 0), stop=False), [cbb, HR], [yps])
                vop("pe", lambda E, s_=s_, HI=HI, yps=yps: E.matmul(yps[:, 0:L], cbb[:, 2 * s_ + 1, :], HI[:], start=False, stop=(s_ == 3)), [cbb, HI], [yps])
            vop("dve", lambda E, U=U, yps=yps: E.scalar_tensor_tensor(W["y"][:], U[:], dsk[:, 0:1], yps[:, 0:L], ALU.mult, ALU.add), [U, dsk, yps], [W["y"]])
            tt_op("pool", W["x2"][:], W["y"][:], W["y"][:], ALU.mult, [W["y"]], [W["x2"]])
            vop("pool", lambda E: E.tensor_scalar(W["x2"][:], W["x2"][:], 0.044715, 1.0, ALU.mult, ALU.add), [W["x2"]], [W["x2"]])
            tt_op("pool", W["x2"][:], W["x2"][:], W["y"][:], ALU.mult, [W["x2"], W["y"]], [W["x2"]])
            vop("act", lambda E: E.activation(W["sg"][:], W["x2"][:], AF.Sigmoid, scale=2.0 * float(np.sqrt(2.0 / np.pi))), [W["x2"]], [W["sg"]])
            YO = yo[c_ % 2]
            tt_op("pool", YO[:], W["y"][:], W["sg"][:], ALU.mult, [W["y"], W["sg"]], [YO])
            p.dma("sp", ysT[:, cols], YO[:], YO, reads=[YO])
        p.phase_end()
    if any(p.q[e] for e in ENGS):
        p.emit()
    return nc, None

FH = FFN // 2
FHB = FH // 128


def build_C(tcore, final):
    nc = bass.Bass("TRN2", target_bir_lowering=False)
    di = lambda n, s, dt=F32: nc.dram_tensor(n, s, dt, kind="ExternalInput").ap()
    hT_d = di("hT", [D, tcore])
    glT_d = di("glT", [256, tcore], BF16)
    odT_d = di("odT", [2048, tcore], BF16)
    orT_d = di("orT", [1024, tcore], BF16)
    ysT_d = di("ysT", [1024, tcore], BF16)
    pT_d = di("pT", [256, tcore])
    gains_d = di("gains", [128, 3 * KC])
    w_glu = di("w_glu", [1024, 2048]); w_gu = di("w_gu", [256, 3 * D])
    w_bd = di("w_bd", [2048, D]); w_br = di("w_br", [1024, D]); w_bs = di("w_bs", [1024, D])
    w_o = di("w_o", [D, D]); w_fg = di("w_fg", [D, FFN]); w_fu = di("w_fu", [D, FFN]); w_fd = di("w_fd", [FFN, D])
    w_ple = di("w_ple", [256, D]); w_pgd = di("w_pgd", [D, 256]); w_pgu = di("w_pgu", [256, D])
    outT_d = nc.dram_tensor("outT", [D, tcore], F32, kind="ExternalOutput").ap()

    p = Prog(nc)
    cx = Ctx(p)
    tmp = norm_tmp(p)
    hT = p.sb("hT_sb", [128, KC, TT], F32, ncells=KC)
    big = p.sb("big", [128, FHB, TT], BF16, ncells=FHB)
    x32 = p.sb("x32", [128, KC, TT], BF16, ncells=KC)
    glT = p.sb("glT_sb", [128, 2, TT], BF16, ncells=2)
    pT = p.sb("pT_sb", [128, 2, TT], BF16, ncells=2)
    gdT = p.sb("gdT_sb", [128, 2, TT], BF16, ncells=2)
    gains = p.sb("gains_sb", [128, 3 * KC], F32)
    sg = [p.sb(f"sg{i}", [128, TT], F32) for i in range(4)]
    tt = [p.sb(f"tt{i}", [128, TT], F32) for i in range(2)]
    macc = [p.sb(f"macc{i}", [128, TT], F32) for i in range(4)]
    p.dma("sp", gains[:], gains_d, gains, writes=[gains])
    g_ffn = gains[:, 0:KC]; g_ple = gains[:, KC:2 * KC]; g_fin = gains[:, 2 * KC:3 * KC]
    tti = [0]

    def nexttt():
        tti[0] += 1
        return tt[tti[0] % 2]

    def bigrhs(base):
        return lambda kc: (big[:, base + kc, :], [big.cells[base + kc]])

    def act_to(func, dst, src):
        p.op("act", lambda E: E.activation(dst[:], src[:, :], func), reads=[src], writes=[dst])

    for t in range(tcore // TT):
        t0 = t * TT
        cols = slice(t0, t0 + TT)
        for q in range(4):
            src = hT_d[q * 1024:(q + 1) * 1024, cols].rearrange("(k p) t -> p k t", p=128)
            p.dma("sp", hT[:, q * 8:(q + 1) * 8, :], src, hT, writes=hT.cells[q * 8:(q + 1) * 8])
        for (src_d, base, n) in ((odT_d, 0, 16), (orT_d, 16, 8), (ysT_d, 24, 8)):
            for q in range(0, n, 8):
                src = src_d[q * 128:(q + 8) * 128, cols].rearrange("(k p) t -> p k t", p=128)
                p.dma("sp", big[:, base + q:base + q + 8, :], src, big, writes=big.cells[base + q:base + q + 8])
        p.dma("sp", glT[:, :, :], glT_d[:, cols].rearrange("(k p) t -> p k t", p=128), glT, writes=[glT])
        p.dma("pool", pT[:, :, :], pT_d[:, cols].rearrange("(k p) t -> p k t", p=128), pT, writes=[pT])

        for n0 in (0, 512):
            A = cx.bankset(); wjob(cx, w_glu, 0, 8, n0, 512, bigrhs(24), A)
            B = cx.bankset(); wjob(cx, w_glu, 0, 8, 1024 + n0, 512, bigrhs(24), B)
            for j in range(4):
                act_to(AF.Sigmoid, sg[j], B[j])
                c = 32 + n0 // 128 + j
                p.op("dve", lambda E, o=big[:, c, :], a=A[j][:, :], s=sg[j][:]: E.tensor_tensor(o, a, s, ALU.mult),
                     reads=[A[j], sg[j]], writes=[big.cells[c]])

        for n0 in range(0, D, 512):
            for b, (Wb, nk, base) in enumerate(((w_bd, 16, 0), (w_br, 8, 16), (w_bs, 8, 32))):
                G = cx.bankset()
                wjob(cx, w_gu, 0, 2, b * D + n0, 512, lambda kc: (glT[:, kc, :], [glT.cells[kc]]), G)
                B = cx.bankset()
                wjob(cx, Wb, 0, nk, n0, 512, bigrhs(base), B)
                for j in range(4):
                    act_to(AF.Sigmoid, sg[j], G[j])
                    c = n0 // 128 + j
                    if b == 0:
                        p.op("dve", lambda E, o=macc[j][:], a=B[j][:, :], s=sg[j][:]: E.tensor_tensor(o, a, s, ALU.mult),
                             reads=[B[j], sg[j]], writes=[macc[j]])
                    else:
                        x = nexttt()
                        p.op("dve", lambda E, o=x[:], a=B[j][:, :], s=sg[j][:]: E.tensor_tensor(o, a, s, ALU.mult),
                             reads=[B[j], sg[j]], writes=[x])
                        if b == 1:
                            p.op("pool", lambda E, o=macc[j][:], a=macc[j][:], s=x[:]: E.tensor_tensor(o, a, s, ALU.add),
                                 reads=[macc[j], x], writes=[macc[j]])
                        else:
                            p.op("pool", lambda E, o=x32[:, c, :], a=macc[j][:], s=x[:]: E.tensor_tensor(o, a, s, ALU.add),
                                 reads=[macc[j], x], writes=[x32.cells[c]])

        def resid_job(W, r0, nk, rhs):
            for n0 in range(0, D, 512):
                B = cx.bankset()
                wjob(cx, W, r0, nk, n0, 512, rhs, B)
                for j in range(4):
                    c = n0 // 128 + j
                    p.op("dve", lambda E, o=hT[:, c, :], a=B[j][:, :]: E.tensor_tensor(o, o, a, ALU.add),
                         reads=[B[j], hT.cells[c]], writes=[hT.cells[c]])

        xrhs = lambda kc: (x32[:, kc, :], [x32.cells[kc]])
        resid_job(w_o, 0, KC, xrhs)
        rmsnorm_T(cx, hT, g_ffn_t(gains, 0), x32, tmp)
        for half in range(2):
            hb = half * FH
            for n0 in range(0, FH, 512):
                w = min(512, FH - n0)
                G = cx.bankset(); wjob(cx, w_fg, 0, KC, hb + n0, w, xrhs, G)
                U = cx.bankset(); wjob(cx, w_fu, 0, KC, hb + n0, w, xrhs, U)
                for j in range(w // 128):
                    act_to(AF.Silu, sg[j], G[j])
                    c = n0 // 128 + j
                    p.op("dve", lambda E, o=big[:, c, :], a=U[j][:, :], s=sg[j][:]: E.tensor_tensor(o, a, s, ALU.mult),
                         reads=[U[j], sg[j]], writes=[big.cells[c]])
            resid_job(w_fd, hb, FHB, bigrhs(0))
        rmsnorm_T(cx, hT, g_ffn_t(gains, 1), x32, tmp)
        Gd = cx.bankset()
        wjob(cx, w_pgd, 0, KC, 0, 256, xrhs, Gd)
        for j in range(2):
            copy_op(p, "act", gdT[:, j, :], Gd[j][:, :], [Gd[j]], [gdT.cells[j]])
        for n0 in range(0, D, 512):
            G = cx.bankset(); wjob(cx, w_pgu, 0, 2, n0, 512, lambda kc: (gdT[:, kc, :], [gdT.cells[kc]]), G)
            P = cx.bankset(); wjob(cx, w_ple, 0, 2, n0, 512, lambda kc: (pT[:, kc, :], [pT.cells[kc]]), P)
            for j in range(4):
                act_to(AF.Sigmoid, sg[j], G[j])
                c = n0 // 128 + j
                x = nexttt()
                p.op("dve", lambda E, o=x[:], a=P[j][:, :], s=sg[j][:]: E.tensor_tensor(o, a, s, ALU.mult),
                     reads=[P[j], sg[j]], writes=[x])
                p.op("pool", lambda E, o=hT[:, c, :], s=x[:]: E.tensor_tensor(o, o, s, ALU.add),
                     reads=[hT.cells[c], x], writes=[hT.cells[c]])
        if final:
            rmsnorm_T(cx, hT, g_ffn_t(gains, 2), hT, tmp)
        for q in range(4):
            dst = outT_d[q * 1024:(q + 1) * 1024, cols].rearrange("(k p) t -> p k t", p=128)
            p.dma("sp", dst, hT[:, q * 8:(q + 1) * 8, :], hT, reads=hT.cells[q * 8:(q + 1) * 8])
    st = p.emit()
    return nc, st


class g_ffn_t:
    def __init__(self, gains, i):
        self.g = gains
        self.i = i
        self.cells = gains.cells

    def __getitem__(self, k):
        rows, colsl = k
        return self.g.t[rows, self.i * KC + colsl.start: self.i * KC + colsl.stop]

import math


def _t5_bucket_np(rel):
    n = np.maximum(rel, 0)
    nf = np.maximum(n, 1).astype(np.float32)
    large = 16 + (np.log(nf / np.float32(16)) / np.float32(math.log(8.0)) * np.float32(16)).astype(np.int32)
    large = np.minimum(large, 31)
    return np.where(n < 16, n, large)


def rep128(v):
    return np.ascontiguousarray(np.broadcast_to(np.asarray(v, np.float32).reshape(1, -1), (128, np.asarray(v).size)))


def b_consts(S):
    out = {}
    p_ = np.arange(128)[:, None, None]; j_ = np.arange(5)[None, :, None]; q_ = np.arange(512)[None, None, :]
    rel = q_ - p_ - 128 * (j_ - 1)
    out["rel"] = rel
    out["bucket"] = _t5_bucket_np(rel)
    pos = np.arange(S, dtype=np.float32)
    theta = (1.0 / (10000.0 ** np.linspace(0.0, 1.0, 64, dtype=np.float32))).astype(np.float32)
    ang = (pos[:, None] * theta[None, :]).astype(np.float32).astype(np.float64)
    c = np.cos(ang).T.astype(np.float32); s = np.sin(ang).T.astype(np.float32)
    out["cosF"] = np.ascontiguousarray(np.concatenate([c, c], 0))
    out["sinS"] = np.ascontiguousarray(np.concatenate([-s, s], 0))
    out["ident"] = np.eye(128, dtype=np.float32)
    g_ = np.arange(128) // 16
    out["bmask"] = np.ascontiguousarray((g_[:, None] == (np.arange(512) // 64)[None, :]).astype(np.float32))
    out["iota"] = rep128(np.arange(S5L + 1, dtype=np.float32))
    rc = []
    cv45 = []
    for h in range(8):
        lg = math.log1p(-2.0 ** (-5.0 - h))
        i_ = np.arange(128)
        d = i_[None, :] - i_[:, None]
        dm = np.where(d >= 0, np.exp(lg * np.maximum(d, 0)), 0.0) * SCALE
        xi = np.exp(lg * (i_ + 1.0))
        rc.append(np.concatenate([dm, np.broadcast_to(np.tile(xi, 4)[None, :], (128, 512))], 1).astype(np.float32))
        zeta = np.exp(lg * (127.0 - i_)) * SCALE
        cv45.append((zeta.astype(np.float32), np.float32(math.exp(lg * 128))))
    out["rconst"] = rc
    out["cv45"] = cv45
    return out


def b_inputs(layer, h, S, K, rel_bias, diff_lambda, diff_subln, s5p):
    lam_init = 0.8 - 0.6 * math.exp(-0.3 * layer)
    d = {}
    gathered = rel_bias[K["bucket"], h].astype(np.float32)
    d["btile"] = np.ascontiguousarray(np.where(K["rel"] >= 0, gathered, np.float32(-30000.0)).astype(np.float32))
    cv = np.zeros((128, 8), np.float32)
    cv[:, 0] = rel_bias[31, h]; cv[:, 1] = lam_init; cv[:, 2] = 1.0 - lam_init; cv[:, 3] = EPS
    cv[:, 4] = K["cv45"][h][0]; cv[:, 5] = K["cv45"][h][1]
    d["cvec"] = cv
    d["dlam"] = rep128(diff_lambda[layer].reshape(-1))
    d["subln"] = np.ascontiguousarray(diff_subln[layer].reshape(2, 128).T)
    d["cosF"] = K["cosF"]; d["sinS"] = K["sinS"]; d["rconst"] = K["rconst"][h]; d["ident"] = K["ident"]
    lre, lim, ldt, bre, bim, cre, cim, dsk = s5p
    g0 = 8 * h
    bj = np.zeros((128, 257), np.float32)
    for g in range(8):
        rows = slice(g * 16, (g + 1) * 16)
        bj[rows, 0:64] = lre[g0 + g][None, :]
        bj[rows, 64:128] = lim[g0 + g][None, :]
        bj[rows, 128:192] = bre[g0 + g].T
        bj[rows, 192:256] = bim[g0 + g].T
        bj[rows, 256] = ldt[g0 + g]
    d["s5bj"] = bj
    d["bmask"] = K["bmask"]
    st = np.zeros((128, 12), np.float32)
    cb = np.zeros((128, 8, 128), np.float32)
    for s_ in range(4):
        for gp in range(2):
            g = 2 * s_ + gp
            rows = slice(gp * 64, (gp + 1) * 64)
            st[rows, s_] = lre[g0 + g]; st[rows, 4 + s_] = lim[g0 + g]; st[rows, 8 + s_] = ldt[g0 + g]
            cb[rows, 2 * s_, g * 16:(g + 1) * 16] = cre[g0 + g].T
            cb[rows, 2 * s_ + 1, g * 16:(g + 1) * 16] = cim[g0 + g].T
    d["s5st"] = st; d["cblk"] = cb
    d["s5d"] = np.ascontiguousarray(dsk[128 * h:128 * (h + 1)].reshape(128, 1).astype(np.float32))
    d["iota"] = K["iota"]
    return d

from concourse.bass_utils import run_bass_kernel_spmd
import ml_dtypes

NCORES = 8
_PROGS = {}


def _prog(key, fn):
    if key not in _PROGS:
        _PROGS[key] = fn()[0]
    return _PROGS[key]


def _gl(g):
    return np.ascontiguousarray(np.asarray(g, np.float32).reshape(KC, 128).T)


def _c(a):
    return np.ascontiguousarray(a)


def kernel(x, p, rel_bias, norm_mix, w_in, diff_lambda, diff_subln,
           s5_lambda_re, s5_lambda_im, s5_log_dt, s5_b_re, s5_b_im, s5_c_re, s5_c_im,
           s5_d, s5_w_glu, w_gate_up, w_br_diff, w_br_ret, w_br_s5, w_o,
           norm_ffn, w_ffn_gate, w_ffn_up, w_ffn_down,
           norm_ple, w_ple, w_ple_gate_down, w_ple_gate_up, norm_final):
    f = lambda a: np.asarray(a, np.float32)
    x = f(x); p = f(p); rel_bias = f(rel_bias)
    S = x.shape[1]
    depth = int(np.asarray(w_in).shape[0])
    tcore = S // NCORES
    cores = list(range(NCORES))
    pa = _prog(("A", tcore), lambda: build_A(tcore))
    pb = _prog(("B", S), lambda: build_B(S))
    K = b_consts(S)
    hT = [_c(x[0, c * tcore:(c + 1) * tcore].T) for c in cores]
    for i in range(depth):
        wi = f(w_in[i]); g = _gl(norm_mix[i])
        ra = run_bass_kernel_spmd(pa, [{"hT": hT[c], "g": g, "w_in": wi} for c in cores], core_ids=cores).results
        zT = np.concatenate([r["zT"] for r in ra], axis=1)
        zv = np.concatenate([r["zv"] for r in ra], axis=0)
        s5p = tuple(f(a[i]) for a in (s5_lambda_re, s5_lambda_im, s5_log_dt, s5_b_re, s5_b_im, s5_c_re, s5_c_im, s5_d))
        ims = []
        for h in cores:
            d = b_inputs(i, h, S, K, rel_bias, f(diff_lambda), f(diff_subln), s5p)
            d["dqT"] = _c(zT[h * 256:(h + 1) * 256]); d["dkT"] = _c(zT[2048 + h * 256:2048 + (h + 1) * 256])
            d["dv"] = _c(zv[:, h * 256:(h + 1) * 256])
            d["rqT"] = _c(zT[4096 + h * 128:4096 + (h + 1) * 128]); d["rkT"] = _c(zT[5120 + h * 128:5120 + (h + 1) * 128])
            d["rv"] = _c(zv[:, 2048 + h * 128:2048 + (h + 1) * 128])
            d["rgT"] = _c(zT[6144 + h * 128:6144 + (h + 1) * 128]); d["suT"] = _c(zT[7168 + h * 128:7168 + (h + 1) * 128])
            ims.append(d)
        rb = run_bass_kernel_spmd(pb, ims, core_ids=cores).results
        odT = np.concatenate([r["odT"] for r in rb], axis=0)
        orT = np.concatenate([r["orT"] for r in rb], axis=0)
        ysT = np.concatenate([r["ysT"] for r in rb], axis=0)
        del rb
        final = (i == depth - 1)
        pc = _prog(("C", tcore, final), lambda: build_C(tcore, final))
        gains = _c(np.concatenate([_gl(norm_ffn[i]), _gl(norm_ple[i]), _gl(norm_final)], axis=1))
        W = dict(w_glu=f(s5_w_glu[i]), w_gu=f(w_gate_up[i]), w_bd=f(w_br_diff[i]), w_br=f(w_br_ret[i]), w_bs=f(w_br_s5[i]),
                 w_o=f(w_o[i]), w_fg=f(w_ffn_gate[i]), w_fu=f(w_ffn_up[i]), w_fd=f(w_ffn_down[i]),
                 w_ple=f(w_ple[i]), w_pgd=f(w_ple_gate_down[i]), w_pgu=f(w_ple_gate_up[i]))
        ims = []
        for c in cores:
            ts = slice(c * tcore, (c + 1) * tcore)
            d = dict(hT=hT[c], glT=_c(zT[8192:8448, ts]), odT=_c(odT[:, ts]), orT=_c(orT[:, ts]), ysT=_c(ysT[:, ts]),
                     pT=_c(p[i, 0, ts].T), gains=gains)
            d.update(W)
            ims.append(d)
        del zT, zv
        rc = run_bass_kernel_spmd(pc, ims, core_ids=cores).results
        hT = [r["outT"] for r in rc]
        del rc, ims
    out = np.concatenate([h_.T for h_ in hT], axis=0)[None]
    return np.ascontiguousarray(out.astype(np.float32))
```
